# Optimizing a Trainium2 kernel written in Bass

```python
import math
import jax, jax.numpy as jnp
from jax import lax
import numpy as np

D_MODEL = 1024
BATCH = 4
SEQ = 8192
DEPTH = 4

N_MIXERS = 2
DA_HEADS = 8
DA_HEAD_DIM = 64
DA_V_DIM = 2 * DA_HEAD_DIM
DA_Q_BLOCK = 128
DA_LAMBDA_STD = 0.1
RET_HEADS = 4
RET_QK_DIM = D_MODEL // RET_HEADS
RET_V_DIM = 2 * RET_QK_DIM
RET_CHUNK = 128
N_EXPERTS = 32
TOP_K = 4
D_FF = D_MODEL
SWIGLU_LIMIT = 7.0
SWIGLU_ALPHA = 1.702
MOE_BLOCK = 128
ROUTER_BIAS_STD = 0.01
LN_EPS = 1e-5
DEEPNORM_ALPHA = (2.0 * DEPTH) ** 0.25
DEEPNORM_BETA = (8.0 * DEPTH) ** -0.25
N_DA_LAYERS = (DEPTH + 1) // 2
N_RET_LAYERS = DEPTH // 2

kernel_name = "hybrid_diffattn_retnet_moe_deepnorm"


def layer_norm(x, g, b):
    xf = x.astype(jnp.float32)
    mu = jnp.mean(xf, axis=-1, keepdims=True)
    var = jnp.mean(jnp.square(xf - mu), axis=-1, keepdims=True)
    return ((xf - mu) * lax.rsqrt(var + LN_EPS)).astype(x.dtype) * g + b


def group_norm_no_affine(x):
    xf = x.astype(jnp.float32)
    mu = jnp.mean(xf, axis=-1, keepdims=True)
    var = jnp.mean(jnp.square(xf - mu), axis=-1, keepdims=True)
    return (xf - mu) * lax.rsqrt(var + LN_EPS)


def rms_norm(x, g):
    xf = x.astype(jnp.float32)
    ms = jnp.mean(jnp.square(xf), axis=-1, keepdims=True)
    return (xf * lax.rsqrt(ms + LN_EPS)).astype(x.dtype) * g


def alibi_slopes(n_heads):
    return jnp.asarray(2.0 ** (-8.0 * np.arange(1, n_heads + 1) / n_heads), dtype=jnp.float32)


def diff_attention(x, w_in, w_out, lam_q1, lam_k1, lam_q2, lam_k2, subln_g, lambda_init):
    B, S, _ = x.shape
    H, dh, dv, QB = DA_HEADS, DA_HEAD_DIM, DA_V_DIM, DA_Q_BLOCK
    nq = S // QB
    qkv = x @ w_in
    q, k, v = jnp.split(qkv, 3, axis=-1)
    q = q.reshape(B, S, H, 2, dh) * (dh ** -0.5)
    k = k.reshape(B, S, H, 2, dh)
    v = v.reshape(B, S, H, dv)
    lam = (jnp.exp(jnp.sum(lam_q1 * lam_k1).astype(jnp.float32))
           - jnp.exp(jnp.sum(lam_q2 * lam_k2).astype(jnp.float32)) + lambda_init)
    slopes = alibi_slopes(H)[:, None, None, None]
    key_pos = jnp.arange(S)
    q_blocks = jnp.moveaxis(q.reshape(B, nq, QB, H, 2, dh), 1, 0)

    def one_block(args):
        qb, blk = args
        q_pos = blk * QB + jnp.arange(QB)
        s = jnp.einsum('bqhmd,bkhmd->bhmqk', qb, k).astype(jnp.float32)
        dist = (q_pos[:, None] - key_pos[None, :]).astype(jnp.float32)
        bias = jnp.where(dist >= 0, -slopes * dist, -jnp.inf)
        p = jax.nn.softmax(s + bias[None], axis=-1)
        a = p[:, :, 0] - lam * p[:, :, 1]
        return jnp.einsum('bhqk,bkhd->bqhd', a.astype(v.dtype), v)

    o = lax.map(one_block, (q_blocks, jnp.arange(nq)))
    o = jnp.moveaxis(o, 0, 1).reshape(B, S, H, dv)
    o = rms_norm(o, subln_g) * (1.0 - lambda_init)
    return o.reshape(B, S, H * dv) @ w_out


def retention(x, w_in, w_out):
    B, S, _ = x.shape
    H, dk, dv, C = RET_HEADS, RET_QK_DIM, RET_V_DIM, RET_CHUNK
    n = S // C
    proj = x @ w_in
    q, k, v, g = jnp.split(proj, [H * dk, 2 * H * dk, 2 * H * dk + H * dv], axis=-1)
    q = q.reshape(B, n, C, H, dk)
    k = k.reshape(B, n, C, H, dk) * (dk ** -0.5)
    v = v.reshape(B, n, C, H, dv)
    log_gamma = jnp.log1p(-jnp.exp2(-5.0 - jnp.arange(H, dtype=jnp.float32)))
    pos = jnp.arange(C, dtype=jnp.float32)
    diff = pos[:, None] - pos[None, :]
    decay_mask = jnp.where(diff >= 0, jnp.exp(log_gamma[:, None, None] * jnp.maximum(diff, 0.0)), 0.0)
    sc = jnp.einsum('bnqhd,bnkhd->bnhqk', q, k) * decay_mask[None, None]
    inner = jnp.einsum('bnhqk,bnkhe->bnqhe', sc, v)
    q_decay = jnp.exp(log_gamma[None, :] * (pos[:, None] + 1.0))
    k_decay = jnp.exp(log_gamma[None, :] * (C - 1.0 - pos[:, None]))
    chunk_decay = jnp.exp(log_gamma * C)

    def step(state, inp):
        qc, kc, vc = inp
        cross = jnp.einsum('bqhd,bhde->bqhe', qc, state) * q_decay[None, :, :, None]
        state = (state * chunk_decay[None, :, None, None]
                 + jnp.einsum('bkhd,bkhe->bhde', kc * k_decay[None, :, :, None], vc))
        return state, cross

    init = jnp.zeros((B, H, dk, dv), jnp.float32)
    _, cross = lax.scan(step, init, (jnp.moveaxis(q, 1, 0), jnp.moveaxis(k, 1, 0), jnp.moveaxis(v, 1, 0)))
    o = inner + jnp.moveaxis(cross, 0, 1)
    o = group_norm_no_affine(o.reshape(B, S, H, dv)).astype(x.dtype)
    o = jax.nn.silu(g) * o.reshape(B, S, H * dv)
    return o @ w_out


def moe(x, w_router, b_router, w_gate_up, b_gate_up, w_down, b_down):
    B, S, D = x.shape
    T = B * S
    E, BLK = N_EXPERTS, MOE_BLOCK
    xt = x.reshape(T, D)
    logits = (xt @ w_router + b_router).astype(jnp.float32)
    top_vals, top_idx = lax.top_k(logits, TOP_K)
    top_w = jax.nn.softmax(top_vals, axis=-1)
    n_assign = T * TOP_K
    expert_of = top_idx.reshape(-1).astype(jnp.int32)
    token_of = jnp.arange(n_assign, dtype=jnp.int32) // TOP_K
    gate_of = top_w.reshape(-1)
    order = jnp.argsort(expert_of)
    sorted_expert = expert_of[order]
    counts = jnp.zeros((E,), jnp.int32).at[expert_of].add(1)
    group_start = jnp.cumsum(counts) - counts
    padded = (counts + BLK - 1) // BLK * BLK
    padded_end = jnp.cumsum(padded)
    padded_start = padded_end - padded
    rank = jnp.arange(n_assign, dtype=jnp.int32) - group_start[sorted_expert]
    dest = padded_start[sorted_expert] + rank
    n_slots = n_assign + E * BLK
    n_blocks = n_slots // BLK
    slot_token = jnp.zeros((n_slots,), jnp.int32).at[dest].set(token_of[order])
    slot_gate = jnp.zeros((n_slots,), jnp.float32).at[dest].set(gate_of[order])
    block_expert = jnp.minimum(
        jnp.searchsorted(padded_end, jnp.arange(n_blocks, dtype=jnp.int32) * BLK, side='right'), E - 1)
    x_slots = xt[slot_token].reshape(n_blocks, BLK, D)

    def expert_block(args):
        xb, e = args
        h = xb @ w_gate_up[e] + b_gate_up[e]
        gate, up = jnp.split(h, 2, axis=-1)
        gate = jnp.minimum(gate, SWIGLU_LIMIT)
        up = jnp.clip(up, -SWIGLU_LIMIT, SWIGLU_LIMIT)
        act = gate * jax.nn.sigmoid(SWIGLU_ALPHA * gate) * (up + 1.0)
        return act @ w_down[e] + b_down[e]

    y_slots = lax.map(expert_block, (x_slots, block_expert)).reshape(n_slots, D)
    y = jax.ops.segment_sum(y_slots * slot_gate[:, None].astype(y_slots.dtype), slot_token, num_segments=T)
    return y.reshape(B, S, D)


def setup_inputs(seed: int = 0) -> dict:
    key = jax.random.key(seed)
    ks = jax.random.split(key, 24)
    f32 = jnp.float32
    D, F, E = D_MODEL, D_FF, N_EXPERTS
    nrm = lambda k, shape, s: jax.random.normal(k, shape, f32) * s
    x = nrm(ks[0], (BATCH, SEQ, D), 1.0)
    da_qk = nrm(ks[1], (N_DA_LAYERS, D, 2 * DA_HEADS * 2 * DA_HEAD_DIM), D ** -0.5)
    da_v = nrm(ks[2], (N_DA_LAYERS, D, DA_HEADS * DA_V_DIM), D ** -0.5 * DEEPNORM_BETA)
    da_w_in = jnp.concatenate([da_qk, da_v], axis=-1)
    da_w_out = nrm(ks[3], (N_DA_LAYERS, DA_HEADS * DA_V_DIM, D), (DA_HEADS * DA_V_DIM) ** -0.5 * DEEPNORM_BETA)
    da_lam_q1 = nrm(ks[4], (N_DA_LAYERS, DA_HEAD_DIM), DA_LAMBDA_STD)
    da_lam_k1 = nrm(ks[5], (N_DA_LAYERS, DA_HEAD_DIM), DA_LAMBDA_STD)
    da_lam_q2 = nrm(ks[6], (N_DA_LAYERS, DA_HEAD_DIM), DA_LAMBDA_STD)
    da_lam_k2 = nrm(ks[7], (N_DA_LAYERS, DA_HEAD_DIM), DA_LAMBDA_STD)
    da_subln_g = 1.0 + nrm(ks[8], (N_DA_LAYERS, DA_V_DIM), 0.02)
    ret_qk = nrm(ks[9], (N_RET_LAYERS, D, 2 * RET_HEADS * RET_QK_DIM), D ** -0.5)
    ret_v = nrm(ks[10], (N_RET_LAYERS, D, RET_HEADS * RET_V_DIM), D ** -0.5 * DEEPNORM_BETA)
    ret_g = nrm(ks[11], (N_RET_LAYERS, D, RET_HEADS * RET_V_DIM), D ** -0.5)
    ret_w_in = jnp.concatenate([ret_qk, ret_v, ret_g], axis=-1)
    ret_w_out = nrm(ks[12], (N_RET_LAYERS, RET_HEADS * RET_V_DIM, D), (RET_HEADS * RET_V_DIM) ** -0.5 * DEEPNORM_BETA)
    moe_w_router = nrm(ks[13], (DEPTH, D, E), D ** -0.5)
    moe_b_router = nrm(ks[14], (DEPTH, E), ROUTER_BIAS_STD)
    moe_w_gate_up = nrm(ks[15], (DEPTH, E, D, 2 * F), D ** -0.5)
    moe_b_gate_up = nrm(ks[16], (DEPTH, E, 2 * F), 0.01)
    moe_w_down = nrm(ks[17], (DEPTH, E, F, D), F ** -0.5 * DEEPNORM_BETA)
    moe_b_down = nrm(ks[18], (DEPTH, E, D), 0.01)
    ln_mix_g = 1.0 + nrm(ks[19], (DEPTH, D), 0.02)
    ln_mix_b = nrm(ks[20], (DEPTH, D), 0.02)
    ln_ffn_g = 1.0 + nrm(ks[21], (DEPTH, D), 0.02)
    ln_ffn_b = nrm(ks[22], (DEPTH, D), 0.02)
    return {"x": x, "da_w_in": da_w_in, "da_w_out": da_w_out,
            "da_lam_q1": da_lam_q1, "da_lam_k1": da_lam_k1, "da_lam_q2": da_lam_q2, "da_lam_k2": da_lam_k2,
            "da_subln_g": da_subln_g, "ret_w_in": ret_w_in, "ret_w_out": ret_w_out,
            "moe_w_router": moe_w_router, "moe_b_router": moe_b_router,
            "moe_w_gate_up": moe_w_gate_up, "moe_b_gate_up": moe_b_gate_up,
            "moe_w_down": moe_w_down, "moe_b_down": moe_b_down,
            "ln_mix_g": ln_mix_g, "ln_mix_b": ln_mix_b, "ln_ffn_g": ln_ffn_g, "ln_ffn_b": ln_ffn_b}


def reference(x, da_w_in, da_w_out, da_lam_q1, da_lam_k1, da_lam_q2, da_lam_k2, da_subln_g,
              ret_w_in, ret_w_out, moe_w_router, moe_b_router, moe_w_gate_up, moe_b_gate_up,
              moe_w_down, moe_b_down, ln_mix_g, ln_mix_b, ln_ffn_g, ln_ffn_b):
    for i in range(DEPTH):
        j = i // N_MIXERS
        if i % N_MIXERS == 0:
            lambda_init = 0.8 - 0.6 * math.exp(-0.3 * i)
            h = diff_attention(x, da_w_in[j], da_w_out[j], da_lam_q1[j], da_lam_k1[j],
                               da_lam_q2[j], da_lam_k2[j], da_subln_g[j], lambda_init)
        else:
            h = retention(x, ret_w_in[j], ret_w_out[j])
        x = layer_norm(DEEPNORM_ALPHA * x + h, ln_mix_g[i], ln_mix_b[i])
        f = moe(x, moe_w_router[i], moe_b_router[i], moe_w_gate_up[i], moe_b_gate_up[i],
                moe_w_down[i], moe_b_down[i])
        x = layer_norm(DEEPNORM_ALPHA * x + f, ln_ffn_g[i], ln_ffn_b[i])
    return x
```

```python
import math
from contextlib import ExitStack

import numpy as np
import ml_dtypes

import concourse.bass as bass
import concourse.mybir as mybir
from concourse.bass_utils import run_bass_kernel_spmd

F32 = mybir.dt.float32
BF16 = mybir.dt.bfloat16
I32 = mybir.dt.int32
AF = mybir.ActivationFunctionType
ALU = mybir.AluOpType
AX = mybir.AxisListType

D = 1024
NCORE = 8
SEQ = 8192
BATCH = 4
NT = 4096
NTILE = NT // 128
NE = 32
CAP = 640
NCH = CAP // 128
ALPHA = (2.0 * 4) ** 0.25
LN_EPS = 1e-5
SAME_ENG_SYNC = True
DBG_SKIP = set()


class Prog:
    ENG = ("pe", "act", "dve", "pool", "sp")

    def __init__(self, nc, stack, nch=None):
        self.nc = nc
        nch = nch or {"sp": 16, "act": 4, "pool": 16, "cc": 8}
        self.ccsems = set()
        self.sem = {e: stack.enter_context(nc.semaphore("s_" + e)) for e in self.ENG}
        self.cnt = {e: 0 for e in self.ENG}
        self.ch = {q: [stack.enter_context(nc.semaphore("c_%s%d" % (q, i))) for i in range(n)]
                   for q, n in nch.items()}
        self.chcnt = {q: [0] * n for q, n in nch.items()}
        self.chnext = {q: 0 for q in nch}
        self.semid = {}
        for e in self.ENG:
            self.semid[id(self.sem[e])] = ("E", e)
        self.ops = {e: [] for e in self.ENG}
        self.waited = {e: {} for e in self.ENG}
        self.last_w = {}
        self.readers = {}
        self.nops = 0

    @staticmethod
    def _key(k):
        if isinstance(k, (str, tuple)):
            return k
        return k.name

    def _record(self, eng, fn, r, w, tok, extra):
        deps = {}

        def add(t):
            s, v = t
            if deps.get(id(s), (None, 0))[1] < v:
                deps[id(s)] = (s, v)

        for k in r:
            k = self._key(k)
            if k in self.last_w:
                add(self.last_w[k])
            if isinstance(k, str) and k.startswith("ps"):
                for t in self.readers.get(k, {}).values():
                    add(t)
        for k in w:
            k = self._key(k)
            if k in self.last_w:
                add(self.last_w[k])
            for t in self.readers.get(k, {}).values():
                add(t)
        for t in extra:
            add(t)
        waits = []
        own = self.sem[eng]
        for s, v in deps.values():
            if s is own and (eng == "pe" or not SAME_ENG_SYNC):
                continue
            if self.waited[eng].get(id(s), 0) >= v:
                continue
            self.waited[eng][id(s)] = v
            waits.append((s, v))
        self.ops[eng].append((waits, fn, tok))
        self.nops += 1
        for k in w:
            k = self._key(k)
            self.last_w[k] = tok
            self.readers[k] = {}
        for k in r:
            k = self._key(k)
            d = self.readers.setdefault(k, {})
            s, v = tok
            if d.get(id(s), (None, 0))[1] < v:
                d[id(s)] = tok
        return tok

    def op(self, eng, fn, r=(), w=()):
        self.cnt[eng] += 1
        tok = (self.sem[eng], self.cnt[eng])
        return self._record(eng, fn, r, w, (tok[0], tok[1]), ())

    def dma(self, q, fn, r=(), w=()):
        i = self.chnext[q]
        self.chnext[q] = (i + 1) % len(self.ch[q])
        s = self.ch[q][i]
        extra = []
        if self.chcnt[q][i] > 0:
            extra.append((s, 16 * self.chcnt[q][i]))
        self.chcnt[q][i] += 1
        tok = (s, 16 * self.chcnt[q][i])
        self.cnt
        return self._record(q, fn, r, w, tok, extra)

    def cc(self, fn, r=(), w=()):
        if "cc" not in self.ch:
            raise RuntimeError("no cc channels")
        i = self.chnext["cc"]
        self.chnext["cc"] = (i + 1) % len(self.ch["cc"])
        s = self.ch["cc"][i]
        extra = []
        if self.chcnt["cc"][i] > 0:
            extra.append((s, self.chcnt["cc"][i]))
        self.chcnt["cc"][i] += 1
        tok = (s, self.chcnt["cc"][i])
        self.ccsems.add(id(s))
        return self._record("pool", fn, r, w, tok, extra)

    def all_tokens(self):
        toks = [(self.sem[e], self.cnt[e]) for e in self.ENG if self.cnt[e] > 0]
        for q in self.ch:
            for s, c in zip(self.ch[q], self.chcnt[q]):
                if c > 0:
                    toks.append((s, c if q == "cc" else 16 * c))
        return toks

    def barrier(self):
        toks = self.all_tokens()
        for e in self.ENG:
            waits = []
            for s, v in toks:
                if s is self.sem[e]:
                    continue
                if self.waited[e].get(id(s), 0) >= v:
                    continue
                self.waited[e][id(s)] = v
                waits.append((s, v))
            if waits:
                self.ops[e].append((waits, None, None))
        self.last_w = {}
        self.readers = {}

    def check(self, ops):
        if not hasattr(self, "_simval"):
            self._simval = {}
        val = self._simval
        pc = {e: 0 for e in self.ENG}
        progress = True
        while progress:
            progress = False
            for e in self.ENG:
                while pc[e] < len(ops[e]):
                    waits, fn, tok = ops[e][pc[e]]
                    if any(val.get(id(s), 0) < v for s, v in waits):
                        break
                    if tok is not None:
                        s, v = tok
                        inc = 1 if (id(s) in self.ccsems or any(s is x for x in self.sem.values())) else 16
                        val[id(s)] = val.get(id(s), 0) + inc
                        assert val[id(s)] == v, ("token mismatch", e, pc[e], val[id(s)], v)
                    pc[e] += 1
                    progress = True
        stuck = {e: (pc[e], len(ops[e])) for e in self.ENG if pc[e] < len(ops[e])}
        assert not stuck, ("DEADLOCK", stuck)

    def flush(self):
        nc = self.nc
        ops = self.ops
        self.ops = {e: [] for e in self.ENG}
        self.check(ops)

        def emit(eng_name, e):
            for waits, fn, tok in ops[eng_name]:
                for s, v in waits:
                    e.wait_ge(s, v)
                if fn is None:
                    continue
                ins = fn(e)
                s, v = tok
                if id(s) in self.ccsems:
                    ins.then_inc(s, 1)
                elif eng_name in self.ch and s is not self.sem[eng_name]:
                    ins.then_inc(s, 16)
                else:
                    ins.then_inc(s, 1)

        with nc.Block() as block:
            @block.tensor
            def _(e):
                emit("pe", e)

            @block.scalar
            def _(e):
                emit("act", e)

            @block.vector
            def _(e):
                emit("dve", e)

            @block.gpsimd
            def _(e):
                emit("pool", e)

            @block.sync
            def _(e):
                emit("sp", e)


def _bf16(a):
    return np.asarray(a).astype(ml_dtypes.bfloat16)


def _consts_B():
    c = {}
    c["ident"] = np.eye(128, dtype=np.float32)
    c["utri"] = np.triu(np.ones((128, 128), np.float32), 1)
    c["ones"] = np.ones((128, 128), np.float32)
    c["iota"] = np.tile(np.arange(CAP, dtype=np.float32)[None, :], (128, 1))
    ecap = np.tile((np.arange(NE, dtype=np.float32) * CAP)[None, None, :], (128, NTILE, 1))
    c["ecap"] = ecap.reshape(128, NTILE * NE)
    tp = np.zeros((128, NTILE, 2), np.float32)
    tp[:, :, 0] = np.arange(NTILE)[None, :]
    tp[:, :, 1] = np.arange(128)[:, None]
    c["tp"] = tp.reshape(128, NTILE * 2)
    names = ["ident", "utri", "ones", "iota", "ecap", "tp"]
    offs = {}
    o = 0
    for n in names:
        offs[n] = (o, c[n].shape[1])
        o += c[n].shape[1]
    return np.concatenate([c[n] for n in names], axis=1), offs


def _layer_norm(P, R, OUT, G, Bt, T):
    st, mv, rstd = T["st"], T["mv"], T["rstd"]
    for h in range(2):
        P.op("dve", lambda e, h=h: e.bn_stats(st[:, h * 6:(h + 1) * 6], R[:, h * 512:(h + 1) * 512]),
             r=[R], w=[st])
    P.op("dve", lambda e: e.bn_aggr(mv[:, :], st[:, :]), r=[st], w=[mv])
    P.op("act", lambda e: e.activation(rstd[:, :], mv[:, 1:2], AF.Sqrt, bias=T["eps"][:, 0:1], scale=1.0),
         r=[mv, T["eps"]], w=[rstd])
    P.op("dve", lambda e: e.reciprocal(rstd[:, :], rstd[:, :]), r=[rstd], w=[rstd])
    P.op("dve", lambda e: e.tensor_scalar(OUT[:, :], R[:, :], mv[:, 0:1], rstd[:, 0:1],
                                           ALU.subtract, ALU.mult), r=[R, mv, rstd], w=[OUT])
    P.op("dve", lambda e: e.tensor_tensor(OUT[:, :], OUT[:, :], G[:, :], ALU.mult), r=[OUT, G], w=[OUT])
    P.op("dve", lambda e: e.tensor_tensor(OUT[:, :], OUT[:, :], Bt[:, :], ALU.add), r=[OUT, Bt], w=[OUT])


def build_phaseB(KC, ag=False):
    nc = bass.Bass("TRN2", target_bir_lowering=False)
    cst_np, co = _consts_B()
    NCST = cst_np.shape[1]

    def din(name, shape, dt):
        return nc.dram_tensor(name, shape, dt, kind="ExternalInput").ap()

    xres = din("xres", [NT, D], F32)
    oT = din("oT", [KC, 128, NT], BF16)
    wout = din("wout", [KC * 128, D], F32)
    lnp = din("lnp", [4, D], F32)
    wr = din("wr", [D, NE], F32)
    br = din("br", [1, NE], F32)
    nw = 4 if ag else NE
    wgu = din("wgu", [nw, D, 2 * D], F32)
    bgu = din("bgu", [128, NE * 16], F32)
    wd = din("wd", [nw, D, D], F32)
    bd = din("bd", [NE, D], F32)
    cst = din("cst", [128, NCST], F32)
    xout = nc.dram_tensor("xout", [NT, D], F32, kind="ExternalOutput").ap()
    XM = nc.dram_tensor("XM", [NT, D], F32, kind="Internal").ap()
    Y = nc.dram_tensor("Y", [NE * CAP, D], F32, kind="Internal").ap()
    WB = nc.dram_tensor("WB", [4 * D, 3 * D] if ag else [128, 64], BF16)
    WA = nc.dram_tensor("WA", [NCORE * 4 * D, 3 * D] if ag else [128, 64], BF16)

    with ExitStack() as S0:
        P = Prog(nc, S0)

        def sb(stack, name, shape, dt):
            return stack.enter_context(nc.sbuf_tensor(name, shape, dt))

        def ps(stack, name, shape, dt=F32):
            return stack.enter_context(nc.psum_tensor(name, shape, dt))

        CST = sb(S0, "CST", [128, NCST], F32)
        CSTB = sb(S0, "CSTB", [128, 3 * 128], BF16)
        TPB = sb(S0, "TPB", [128, NTILE * 2], BF16)
        G1 = sb(S0, "G1", [128, D], F32)
        B1 = sb(S0, "B1", [128, D], F32)
        G2 = sb(S0, "G2", [128, D], F32)
        B2 = sb(S0, "B2", [128, D], F32)
        BRt = sb(S0, "BRt", [128, NE], F32)
        EPS = sb(S0, "EPS", [128, 1], F32)
        MASK = sb(S0, "MASK", [128, NTILE * NE], F32)
        GATE = sb(S0, "GATE", [128, NTILE * NE], F32)
        LOG = sb(S0, "LOG", [128, NTILE * NE], F32)
        MX8 = sb(S0, "MX8", [128, NTILE * 8], F32)
        POSM = sb(S0, "POSM", [128, NTILE * NE], F32)
        SLJI = sb(S0, "SLJI", [128, NTILE * 4], I32)
        GJ = sb(S0, "GJ", [128, NTILE * 4], F32)
        TOKI = sb(S0, "TOKI", [128, NE * NCH], I32)
        STGg = [sb(S0, "STGg%d" % i, [128, 2 * D], BF16) for i in range(2)]
        STGd = [sb(S0, "STGd%d" % i, [128, D], BF16) for i in range(2)]
        LT = {k: sb(S0, "ln_" + k, shp, F32) for k, shp in
              [("st", [128, 12]), ("mv", [128, 2]), ("rstd", [128, 1])]}
        LT["eps"] = EPS

        def cs(name, lo=0, hi=None):
            o, n = co[name]
            hi = n if hi is None else hi
            return CST[:, o + lo:o + hi]

        WBa = WB.ap()
        ns = 0
        for le in range(4 if ag else 0):
            for k in range(8):
                sg_, sd_ = STGg[ns % 2], STGd[ns % 2]
                ns += 1
                r0 = le * D + k * 128
                P.dma("pool", lambda e, sg_=sg_, le=le, k=k: e.dma_start(
                    out=sg_[:, :], in_=wgu[le, k * 128:(k + 1) * 128, :]), w=[sg_])
                P.dma("sp", lambda e, sg_=sg_, r0=r0: e.dma_start(out=WBa[r0:r0 + 128, 0:2 * D], in_=sg_[:, :]),
                      r=[sg_], w=["WBd"])
                P.dma("pool", lambda e, sd_=sd_, le=le, k=k: e.dma_start(
                    out=sd_[:, :], in_=wd[le, k * 128:(k + 1) * 128, :]), w=[sd_])
                P.dma("sp", lambda e, sd_=sd_, r0=r0: e.dma_start(out=WBa[r0:r0 + 128, 2 * D:3 * D], in_=sd_[:, :]),
                      r=[sd_], w=["WBd"])
        if ag:
            P.cc(lambda e: e.collective_compute(
                "AllGather", ALU.bypass, replica_groups=[list(range(NCORE))],
                ins=[WB.ap().opt()], outs=[WA.ap().opt()]), r=["WBd"], w=["WAd"])
            P.barrier()
            P.flush()
        P.dma("sp", lambda e: e.dma_start(out=CST[:, :], in_=cst), w=[CST])
        o_id = co["ident"][0]
        P.dma("pool", lambda e: e.dma_start(out=CSTB[:, :], in_=cst[:, o_id:o_id + 384]), w=[CSTB])
        o_tp = co["tp"][0]
        P.dma("pool", lambda e: e.dma_start(out=TPB[:, :], in_=cst[:, o_tp:o_tp + NTILE * 2]), w=[TPB])
        for i, t in enumerate([G1, B1, G2, B2]):
            P.dma("sp", lambda e, i=i, t=t: e.dma_start(out=t[:, :], in_=lnp[i, :].partition_broadcast(128)),
                  w=[t])
        P.dma("sp", lambda e: e.dma_start(out=BRt[:, :], in_=br[0, :].partition_broadcast(128)), w=[BRt])
        P.op("dve", lambda e: e.memset(EPS[:, :], LN_EPS), w=[EPS])
        IDF = cs("ident")
        IDB = CSTB[:, 0:128]
        UTB = CSTB[:, 128:256]
        ONB = CSTB[:, 256:384]

        with ExitStack() as S1:
            WOUT = sb(S1, "WOUT", [128, KC, D], BF16)
            WR = sb(S1, "WR", [128, 8, NE], F32)
            OTs = [sb(S1, "OT%d" % i, [128, KC, 512], BF16) for i in range(2)]
            XR = [sb(S1, "XR%d" % i, [128, D], F32) for i in range(2)]
            R = [sb(S1, "R%d" % i, [128, D], F32) for i in range(2)]
            XMt = [sb(S1, "XMt%d" % i, [128, D], F32) for i in range(2)]
            XMT = [sb(S1, "XMT%d" % i, [128, D], F32) for i in range(2)]
            Lt = sb(S1, "Lt", [128, NE], F32)
            EX = sb(S1, "EX", [128, NE], F32)
            SM = sb(S1, "SM", [128, 4], F32)
            psA = [ps(S1, "psA%d" % i, [128, 512]) for i in range(4)]
            psT = [ps(S1, "psT%d" % i, [128, D]) for i in range(1)]
            psL = ps(S1, "psL", [128, 512])

            for k in range(KC):
                P.dma("pool", lambda e, k=k: e.dma_start(out=WOUT[:, k, :], in_=wout[k * 128:(k + 1) * 128, :]),
                      w=[("WOUT", k)])
            P.dma("sp", lambda e: e.dma_start(out=WR[:, :, :], in_=wr.rearrange("(k p) n -> p k n", p=128)), w=[WR])
            oTv = oT.rearrange("k p t -> p k t")
            for s in range(NT // 512):
                OTb = OTs[s % 2]
                P.dma("sp", lambda e, s=s, OTb=OTb: e.dma_start(out=OTb[:, :, :], in_=oTv[:, :, s * 512:(s + 1) * 512]),
                      w=[OTb])
                for t in range(4):
                    T = s * 4 + t
                    b = T % 2
                    P.dma("sp", lambda e, T=T, b=b: e.dma_start(out=XR[b][:, :], in_=xres[T * 128:(T + 1) * 128, :]),
                          w=[XR[b]])
                    for h in range(2):
                        pa = psA[(T * 2 + h) % 4]
                        for k in range(KC):
                            P.op("pe", lambda e, pa=pa, OTb=OTb, k=k, t=t, h=h: e.matmul(
                                pa[:, :], OTb[:, k, t * 128:(t + 1) * 128], WOUT[:, k, h * 512:(h + 1) * 512],
                                start=(k == 0), stop=(k == KC - 1)),
                                r=[OTb, ("WOUT", k)], w=[pa])
                        P.op("dve", lambda e, pa=pa, b=b, h=h: e.scalar_tensor_tensor(
                            R[b][:, h * 512:(h + 1) * 512], XR[b][:, h * 512:(h + 1) * 512], ALPHA, pa[:, :],
                            ALU.mult, ALU.add), r=[pa, XR[b]], w=[R[b]])
                    _layer_norm(P, R[b], XMt[b], G1, B1, LT)
                    P.dma("sp", lambda e, T=T, b=b: e.dma_start(out=XM[T * 128:(T + 1) * 128, :], in_=XMt[b][:, :]),
                          r=[XMt[b]], w=["XMd"])
                    pT = psT[0]
                    for k in range(8):
                        P.op("pe", lambda e, pT=pT, b=b, k=k: e.transpose(
                            pT[:, k * 128:(k + 1) * 128], XMt[b][:, k * 128:(k + 1) * 128], IDF),
                            r=[XMt[b], CST], w=[pT])
                    P.op("act", lambda e, pT=pT, b=b: e.copy(XMT[b][:, :], pT[:, :]), r=[pT], w=[XMT[b]])
                    for k in range(8):
                        P.op("pe", lambda e, b=b, k=k: e.matmul(
                            psL[:, 0:NE], XMT[b][:, k * 128:(k + 1) * 128], WR[:, k, :],
                            start=(k == 0), stop=(k == 7)), r=[XMT[b], WR], w=[psL])
                    lg = LOG[:, T * NE:(T + 1) * NE]
                    mk = MASK[:, T * NE:(T + 1) * NE]
                    gt = GATE[:, T * NE:(T + 1) * NE]
                    m8 = MX8[:, T * 8:(T + 1) * 8]
                    P.op("dve", lambda e, lg=lg: e.tensor_tensor(lg, psL[:, 0:NE], BRt[:, :], ALU.add),
                         r=[psL, BRt], w=[LOG])
                    P.op("dve", lambda e, lg=lg, m8=m8: e.max(m8, lg), r=[LOG], w=[MX8])
                    P.op("dve", lambda e, lg=lg, m8=m8, mk=mk: e.tensor_scalar(
                        mk, lg, m8[:, 3:4], None, ALU.is_ge), r=[LOG, MX8], w=[MASK])
                    P.op("dve", lambda e, m8=m8: e.tensor_scalar(
                        SM[:, 0:1], m8[:, 0:1], -1.0, None, ALU.mult), r=[MX8], w=[SM])
                    P.op("act", lambda e, lg=lg: e.activation(EX[:, :], lg, AF.Exp, bias=SM[:, 0:1], scale=1.0),
                         r=[LOG, SM], w=[EX])
                    P.op("dve", lambda e, mk=mk: e.tensor_tensor(EX[:, :], EX[:, :], mk, ALU.mult),
                         r=[EX, MASK], w=[EX])
                    P.op("dve", lambda e: e.reduce_sum(SM[:, 1:2], EX[:, :], AX.X), r=[EX], w=[SM])
                    P.op("dve", lambda e: e.reciprocal(SM[:, 2:3], SM[:, 1:2]), r=[SM], w=[SM])
                    P.op("dve", lambda e, gt=gt: e.tensor_scalar(gt, EX[:, :], SM[:, 2:3], None, ALU.mult),
                         r=[EX, SM], w=[GATE])
            P.barrier()
            P.flush()

        with ExitStack() as S2:
            MASKB = sb(S2, "MASKB", [128, NTILE * NE], BF16)
            CNT = sb(S2, "CNT", [128, NTILE * NE], F32)
            OFF = sb(S2, "OFF", [128, NTILE * NE], F32)
            SL = sb(S2, "SL", [128, NTILE * NE], F32)
            SLJ = sb(S2, "SLJ", [128, NTILE * 4], F32)
            TMP = sb(S2, "TMP", [128, NE], F32)
            OH = [sb(S2, "OH%d" % i, [128, CAP], BF16) for i in range(2)]
            TOKF = sb(S2, "TOKF", [128, NE * NCH], F32)
            TK = [sb(S2, "TK%d" % i, [128, 2 * NCH], F32) for i in range(2)]
            psW = [ps(S2, "psW%d" % i, [128, 512]) for i in range(2)]
            psC = [ps(S2, "psC%d" % i, [128, 512]) for i in range(2)]
            psI = [ps(S2, "psI%d" % i, [128, 512]) for i in range(2)]
            ZB = sb(S2, "ZB", [128, 128], BF16)
            P.op("dve", lambda e: e.memset(ZB[:, :], 0.0), w=[ZB])
            P.op("dve", lambda e: e.tensor_copy(MASKB[:, :], MASK[:, :]), r=[MASK], w=[MASKB])
            for h in range(2):
                P.op("pe", lambda e, h=h: e.matmul(psW[h][:, :], UTB, MASKB[:, h * 512:(h + 1) * 512],
                                                    start=True, stop=True), r=[CSTB, MASKB], w=[psW[h]])
                P.op("pe", lambda e, h=h: e.matmul(psC[h][:, :], ONB, MASKB[:, h * 512:(h + 1) * 512],
                                                    start=True, stop=True), r=[CSTB, MASKB], w=[psC[h]])
                P.op("act", lambda e, h=h: e.copy(CNT[:, h * 512:(h + 1) * 512], psC[h][:, :]), r=[psC[h]], w=[CNT])
            P.op("dve", lambda e: e.memset(OFF[:, 0:NE], 0.0), w=[OFF])
            for t in range(1, NTILE):
                P.op("dve", lambda e, t=t: e.tensor_tensor(
                    OFF[:, t * NE:(t + 1) * NE], OFF[:, (t - 1) * NE:t * NE], CNT[:, (t - 1) * NE:t * NE], ALU.add),
                    r=[OFF, CNT], w=[OFF])
            for h in range(2):
                P.op("dve", lambda e, h=h: e.tensor_tensor(
                    POSM[:, h * 512:(h + 1) * 512], psW[h][:, :], OFF[:, h * 512:(h + 1) * 512], ALU.add),
                    r=[psW[h], OFF], w=[POSM])
            P.op("dve", lambda e: e.scalar_tensor_tensor(POSM[:, :], POSM[:, :], 1.0, MASK[:, :], ALU.add, ALU.mult),
                 r=[POSM, MASK], w=[POSM])
            P.op("dve", lambda e: e.tensor_scalar(POSM[:, :], POSM[:, :], -1.0, None, ALU.add), r=[POSM], w=[POSM])
            P.op("dve", lambda e: e.tensor_tensor(SL[:, :], POSM[:, :], cs("ecap"), ALU.add), r=[POSM, CST], w=[SL])
            for T in range(NTILE):
                for j in range(4):
                    c = T * 4 + j
                    P.op("dve", lambda e, T=T, j=j, c=c: e.scalar_tensor_tensor(
                        TMP[:, :], LOG[:, T * NE:(T + 1) * NE], MX8[:, T * 8 + j:T * 8 + j + 1],
                        SL[:, T * NE:(T + 1) * NE], ALU.is_equal, ALU.mult),
                        r=[LOG, MX8, SL], w=[TMP])
                    P.op("dve", lambda e, c=c: e.reduce_sum(SLJ[:, c:c + 1], TMP[:, :], AX.X), r=[TMP], w=[SLJ])
                    P.op("dve", lambda e, T=T, j=j, c=c: e.scalar_tensor_tensor(
                        TMP[:, :], LOG[:, T * NE:(T + 1) * NE], MX8[:, T * 8 + j:T * 8 + j + 1],
                        GATE[:, T * NE:(T + 1) * NE], ALU.is_equal, ALU.mult),
                        r=[LOG, MX8, GATE], w=[TMP])
                    P.op("dve", lambda e, c=c: e.reduce_sum(GJ[:, c:c + 1], TMP[:, :], AX.X), r=[TMP], w=[GJ])
            P.op("dve", lambda e: e.tensor_scalar(SLJ[:, :], SLJ[:, :], float(NE * CAP - 1), 0.0, ALU.min, ALU.max),
                 r=[SLJ], w=[SLJ])
            P.op("dve", lambda e: e.tensor_copy(SLJI[:, :], SLJ[:, :]), r=[SLJ], w=[SLJI])
            n = 0
            for ex in range(NE):
                pI = psI[ex % 2]
                for c in range(NCH):
                    P.op("pe", lambda e, c=c, pI=pI: e.matmul(
                        pI[:, c * 2:c * 2 + 2], ZB[:, :], TPB[:, 0:2], start=True, stop=False), r=[ZB, TPB], w=[pI])
                for t in range(NTILE):
                    oh = OH[n % 2]
                    n += 1
                    P.op("dve", lambda e, oh=oh, t=t, ex=ex: e.tensor_scalar(
                        oh[:, :], cs("iota"), POSM[:, t * NE + ex:t * NE + ex + 1], None, ALU.is_equal),
                        r=[CST, POSM], w=[oh])
                    for c in range(NCH):
                        P.op("pe", lambda e, oh=oh, t=t, c=c, pI=pI: e.matmul(
                            pI[:, c * 2:c * 2 + 2], oh[:, c * 128:(c + 1) * 128], TPB[:, t * 2:t * 2 + 2],
                            start=False, stop=(t == NTILE - 1)), r=[oh, TPB], w=[pI])
                tk = TK[ex % 2]
                P.op("act", lambda e, pI=pI, tk=tk: e.copy(tk[:, :], pI[:, 0:2 * NCH]), r=[pI], w=[tk])
                tkv = tk[:, :].rearrange("p (c two) -> p c two", two=2)
                P.op("dve", lambda e, tkv=tkv, ex=ex: e.scalar_tensor_tensor(
                    TOKF[:, ex * NCH:(ex + 1) * NCH], tkv[:, :, 0], 128.0, tkv[:, :, 1],
                    ALU.mult, ALU.add), r=[tk], w=[TOKF])
            P.op("dve", lambda e: e.tensor_copy(TOKI[:, :], TOKF[:, :]), r=[TOKF], w=[TOKI])
            P.barrier()
            P.flush()

        with ExitStack() as S3:
            WGU = [sb(S3, "WGU%d" % i, [128, 8, 2 * D], BF16) for i in range(2)]
            WD = [sb(S3, "WD%d" % i, [128, 8, D], BF16) for i in range(2)]
            BGU = sb(S3, "BGU", [128, NE * 16], F32)
            BD = [sb(S3, "BD%d" % i, [128, D], F32) for i in range(2)]
            XG = [sb(S3, "XG%d" % i, [128, D], BF16) for i in range(2)]
            XGT = sb(S3, "XGT", [128, 8, CAP], BF16)
            ACTT = sb(S3, "ACTT", [128, 8, CAP], BF16)
            G1t = [sb(S3, "G1t%d" % i, [128, 320], F32) for i in range(2)]
            SGt = [sb(S3, "SGt%d" % i, [128, 320], F32) for i in range(2)]
            U1t = [sb(S3, "U1t%d" % i, [128, 320], F32) for i in range(2)]
            YS = [sb(S3, "YS%d" % i, [128, D], F32) for i in range(2)]
            psX = ps(S3, "psX", [128, D], BF16)
            psG = [ps(S3, "psG%d" % i, [128, 512]) for i in range(2)]
            psU = [ps(S3, "psU%d" % i, [128, 512]) for i in range(2)]
            psY = [ps(S3, "psY%d" % i, [128, 512]) for i in range(2)]
            P.dma("sp", lambda e: e.dma_start(out=BGU[:, :], in_=bgu), w=[BGU])

            order = list(range(NE))
            WAa = WA.ap()

            def load_w(n):
                ex = order[n]
                wb = n % 2
                for k in range(8):
                    r0 = ex * D + k * 128
                    if ag:
                        P.dma("sp", lambda e, k=k, wb=wb, r0=r0: e.dma_start(
                            out=WGU[wb][:, k, :], in_=WAa[r0:r0 + 128, 0:2 * D]), r=["WAd"], w=[("WGU", wb, k)])
                        P.dma("act", lambda e, k=k, wb=wb, r0=r0: e.dma_start(
                            out=WD[wb][:, k, :], in_=WAa[r0:r0 + 128, 2 * D:3 * D]), r=["WAd"], w=[("WD", wb, k)])
                    else:
                        P.dma("pool", lambda e, k=k, wb=wb, ex=ex: e.dma_start(
                            out=WGU[wb][:, k, :], in_=wgu[ex, k * 128:(k + 1) * 128, :]), w=[("WGU", wb, k)])
                        P.dma("pool", lambda e, k=k, wb=wb, ex=ex: e.dma_start(
                            out=WD[wb][:, k, :], in_=wd[ex, k * 128:(k + 1) * 128, :]), w=[("WD", wb, k)])
                P.dma("sp", lambda e, wb=wb, ex=ex: e.dma_start(
                    out=BD[wb][:, :], in_=bd[ex, :].partition_broadcast(128)), w=[BD[wb]])

            load_w(0)
            nact = 0
            for n_ in range(NE):
                ex = order[n_]
                wb = n_ % 2
                for c in range(NCH):
                    xg = XG[c % 2]
                    col = ex * NCH + c
                    P.dma("pool", lambda e, xg=xg, col=col: e.indirect_dma_start(
                        out=xg[:, :], out_offset=None, in_=XM[:, :],
                        in_offset=bass.IndirectOffsetOnAxis(ap=TOKI[:, col:col + 1], axis=0)),
                        r=[TOKI, "XMd"], w=[xg])
                    for k in range(8):
                        P.op("pe", lambda e, xg=xg, k=k: e.transpose(
                            psX[:, k * 128:(k + 1) * 128], xg[:, k * 128:(k + 1) * 128], IDB),
                            r=[xg, CSTB], w=[psX])
                    P.op("act", lambda e, c=c: e.copy(
                        XGT[:, :, c * 128:(c + 1) * 128], psX[:, :].rearrange("p (k t) -> p k t", k=8)),
                        r=[psX], w=[("XGT", c)])
                if n_ + 1 < NE:
                    load_w(n_ + 1)
                for j in range(8):
                    for h in range(2):
                        pg = psG[nact % 2]
                        pu = psU[nact % 2]
                        g1, sg, u1 = G1t[nact % 2], SGt[nact % 2], U1t[nact % 2]
                        nact += 1
                        xr = [("XGT", c) for c in range(NCH)]
                        for k in range(8):
                            P.op("pe", lambda e, pg=pg, wb=wb, k=k, j=j, h=h: e.matmul(
                                pg[:, 0:320], WGU[wb][:, k, j * 128:(j + 1) * 128], XGT[:, k, h * 320:(h + 1) * 320],
                                start=(k == 0), stop=(k == 7)), r=[("WGU", wb, k)] + xr, w=[pg])
                        for k in range(8):
                            P.op("pe", lambda e, pu=pu, wb=wb, k=k, j=j, h=h: e.matmul(
                                pu[:, 0:320], WGU[wb][:, k, D + j * 128:D + (j + 1) * 128],
                                XGT[:, k, h * 320:(h + 1) * 320],
                                start=(k == 0), stop=(k == 7)), r=[("WGU", wb, k)] + xr, w=[pu])
                        bg = BGU[:, ex * 16 + j:ex * 16 + j + 1]
                        bu = BGU[:, ex * 16 + 8 + j:ex * 16 + 8 + j + 1]
                        P.op("dve", lambda e, pg=pg, g1=g1, bg=bg: e.tensor_scalar(
                            g1[:, :], pg[:, 0:320], bg, 7.0, ALU.add, ALU.min), r=[pg, BGU], w=[g1])
                        P.op("act", lambda e, g1=g1, sg=sg: e.activation(sg[:, :], g1[:, :], AF.Sigmoid, scale=1.702),
                             r=[g1], w=[sg])
                        P.op("dve", lambda e, pu=pu, u1=u1, bu=bu: e.tensor_scalar(
                            u1[:, :], pu[:, 0:320], bu, 7.0, ALU.add, ALU.min), r=[pu, BGU], w=[u1])
                        P.op("dve", lambda e, u1=u1: e.tensor_scalar(
                            u1[:, :], u1[:, :], -7.0, 1.0, ALU.max, ALU.add), r=[u1], w=[u1])
                        P.op("dve", lambda e, g1=g1, sg=sg: e.tensor_tensor(g1[:, :], g1[:, :], sg[:, :], ALU.mult),
                             r=[g1, sg], w=[g1])
                        P.op("dve", lambda e, g1=g1, u1=u1, j=j, h=h: e.tensor_tensor(
                            ACTT[:, j, h * 320:(h + 1) * 320], g1[:, :], u1[:, :], ALU.mult),
                            r=[g1, u1], w=[("ACTT", j)])
                for c in range(NCH):
                    ys = YS[c % 2]
                    for h in range(2):
                        py = psY[h]
                        for j in range(8):
                            P.op("pe", lambda e, py=py, wb=wb, j=j, c=c, h=h: e.matmul(
                                py[:, :], ACTT[:, j, c * 128:(c + 1) * 128], WD[wb][:, j, h * 512:(h + 1) * 512],
                                start=(j == 0), stop=(j == 7)), r=[("ACTT", j), ("WD", wb, j)], w=[py])
                        P.op("dve", lambda e, py=py, ys=ys, wb=wb, h=h: e.tensor_tensor(
                            ys[:, h * 512:(h + 1) * 512], py[:, :], BD[wb][:, h * 512:(h + 1) * 512], ALU.add),
                            r=[py, BD[wb]], w=[ys])
                    row = ex * CAP + c * 128
                    P.dma("sp", lambda e, ys=ys, row=row: e.dma_start(out=Y[row:row + 128, :], in_=ys[:, :]),
                          r=[ys], w=["Yd"])
            P.barrier()
            P.flush()

        with ExitStack() as S4:
            YG = [sb(S4, "YG%d" % i, [128, D], F32) for i in range(8)]
            XMr = [sb(S4, "XMr%d" % i, [128, D], F32) for i in range(2)]
            ACC = [sb(S4, "ACC%d" % i, [128, D], F32) for i in range(2)]
            XO = [sb(S4, "XO%d" % i, [128, D], F32) for i in range(2)]
            outs = []
            for T in range(NTILE):
                b = T % 2
                P.dma("sp", lambda e, T=T, b=b: e.dma_start(out=XMr[b][:, :], in_=XM[T * 128:(T + 1) * 128, :]),
                      r=["XMd"], w=[XMr[b]])
                for j in range(4):
                    yg = YG[(T % 2) * 4 + j]
                    c = T * 4 + j
                    P.dma("pool", lambda e, yg=yg, c=c: e.indirect_dma_start(
                        out=yg[:, :], out_offset=None, in_=Y[:, :],
                        in_offset=bass.IndirectOffsetOnAxis(ap=SLJI[:, c:c + 1], axis=0)),
                        r=[SLJI, "Yd"], w=[yg])
                acc = ACC[b]
                P.op("dve", lambda e, acc=acc, b=b: e.tensor_scalar(acc[:, :], XMr[b][:, :], ALPHA, None, ALU.mult),
                     r=[XMr[b]], w=[acc])
                for j in range(4):
                    yg = YG[(T % 2) * 4 + j]
                    c = T * 4 + j
                    P.op("dve", lambda e, acc=acc, yg=yg, c=c: e.scalar_tensor_tensor(
                        acc[:, :], yg[:, :], GJ[:, c:c + 1], acc[:, :], ALU.mult, ALU.add),
                        r=[yg, GJ, acc], w=[acc])
                _layer_norm(P, acc, XO[b], G2, B2, LT)
                tok = P.dma("sp", lambda e, T=T, b=b: e.dma_start(out=xout[T * 128:(T + 1) * 128, :], in_=XO[b][:, :]),
                            r=[XO[b]], w=["xoutd"])
            P.barrier()
            P.flush()
    return nc, cst_np


def _consts_A():
    ident = np.eye(128, dtype=np.float32)
    ones = np.ones((128, 128), np.float32)
    tri = np.triu(np.ones((128, 128), np.float32), 0)
    return np.concatenate([ident, ones, tri], axis=1)


def _alibi_aug(heads):
    pos = np.arange(SEQ)
    hi = (pos // 64).astype(np.float32)
    lo = (pos % 64).astype(np.float32)
    qaug = np.stack([-hi, -lo, np.ones(SEQ, np.float32), np.ones(SEQ, np.float32)])
    kaug = np.zeros((len(heads), 4, SEQ), np.float32)
    for i, h in enumerate(heads):
        slope = 2.0 ** (-8.0 * (h + 1) / 8)
        kaug[i, 0] = slope * 64
        kaug[i, 1] = slope
        kaug[i, 2] = slope * 64 * hi
        kaug[i, 3] = slope * lo
    return _bf16(qaug), _bf16(kaug)


def build_phaseA_da(layer):
    lambda_init = 0.8 - 0.6 * math.exp(-0.3 * layer)
    nc = bass.Bass("TRN2", target_bir_lowering=False)

    def din(name, shape, dt):
        return nc.dram_tensor(name, shape, dt, kind="ExternalInput").ap()

    x = din("x", [SEQ, D], F32)
    wq = din("wq", [D, 512], F32)
    wk = din("wk", [D, 512], F32)
    wv = din("wv", [D, 512], F32)
    lam = din("lam", [4, 64], F32)
    sgin = din("sg", [128, 1], F32)
    qaug = din("qaug", [4, SEQ], BF16)
    kaug = din("kaug", [4, 4, SEQ], BF16)
    cst = din("cstA", [128, 384], F32)
    oT = nc.dram_tensor("oT", [4, 128, SEQ], BF16, kind="ExternalOutput").ap()
    XT = nc.dram_tensor("XT", [8, 128, SEQ], BF16, kind="Internal").ap()
    XTv = XT.rearrange("k p t -> p k t")
    NCHK = SEQ // 512

    with ExitStack() as S0:
        P = Prog(nc, S0)

        def sb(stack, name, shape, dt):
            return stack.enter_context(nc.sbuf_tensor(name, shape, dt))

        def ps(stack, name, shape, dt=F32):
            return stack.enter_context(nc.psum_tensor(name, shape, dt))

        CST = sb(S0, "CST", [128, 384], F32)
        CSTB = sb(S0, "CSTB", [128, 384], BF16)
        P.dma("sp", lambda e: e.dma_start(out=CST[:, :], in_=cst), w=[CST])
        P.dma("pool", lambda e: e.dma_start(out=CSTB[:, :], in_=cst), w=[CSTB])
        IDB, ONB, TRB = CSTB[:, 0:128], CSTB[:, 128:256], CSTB[:, 256:384]
        ONF = CST[:, 128:256]

        with ExitStack() as S1:
            XB = [sb(S1, "XB%d" % i, [128, D], BF16) for i in range(2)]
            XTg = [sb(S1, "XTg%d" % i, [128, 8, 512], BF16) for i in range(2)]
            psX = [ps(S1, "psX%d" % i, [128, D], BF16) for i in range(2)]
            for s in range(NCHK):
                g = XTg[s % 2]
                for t in range(4):
                    T = s * 4 + t
                    xb = XB[T % 2]
                    px = psX[T % 2]
                    P.dma("pool", lambda e, xb=xb, T=T: e.dma_start(out=xb[:, :], in_=x[T * 128:(T + 1) * 128, :]),
                          w=[xb])
                    for k in range(8):
                        P.op("pe", lambda e, px=px, xb=xb, k=k: e.transpose(
                            px[:, k * 128:(k + 1) * 128], xb[:, k * 128:(k + 1) * 128], IDB), r=[xb, CSTB], w=[px])
                    if t % 2 == 0:
                        P.op("act", lambda e, g=g, px=px, t=t: e.copy(
                            g[:, :, t * 128:(t + 1) * 128], px[:, :].rearrange("p (k t) -> p k t", k=8)),
                            r=[px], w=[g])
                    else:
                        P.op("dve", lambda e, g=g, px=px, t=t: e.tensor_copy(
                            g[:, :, t * 128:(t + 1) * 128], px[:, :].rearrange("p (k t) -> p k t", k=8)),
                            r=[px], w=[g])
                P.dma("sp", lambda e, g=g, s=s: e.dma_start(out=XTv[:, :, s * 512:(s + 1) * 512], in_=g[:, :, :]),
                      r=[g], w=["XTd"])
            P.barrier()
            P.flush()

        with ExitStack() as S2:
            WQ = sb(S2, "WQ", [128, 8, 512], BF16)
            WK = sb(S2, "WK", [128, 8, 512], BF16)
            WV = sb(S2, "WV", [128, 8, 512], BF16)
            QA = [sb(S2, "QA%d" % m, [68, SEQ], BF16) for m in range(2)]
            KA = [sb(S2, "KA%d" % m, [68, SEQ], BF16) for m in range(2)]
            V = sb(S2, "V", [128, SEQ // 128, 128], BF16)
            XTc = [sb(S2, "XTc%d" % i, [128, 8, 512], BF16) for i in range(2)]
            PT = [[sb(S2, "PT%d_%d" % (m, i), [128, 512], BF16) for i in range(2)] for m in range(2)]
            LQ = [sb(S2, "LQ%d" % i, [128, 64], F32) for i in range(4)]
            LS = sb(S2, "LS", [128, 4], F32)
            NLAM = sb(S2, "NLAM", [128, 1], F32)
            GSC = sb(S2, "GSC", [128, 1], F32)
            EPS = sb(S2, "EPS", [128, 1], F32)
            R1 = sb(S2, "R1", [128, 512], F32)
            R2 = sb(S2, "R2", [128, 512], F32)
            OA = sb(S2, "OA", [128, 512], F32)
            OB = sb(S2, "OB", [128, 512], F32)
            SQ = sb(S2, "SQ", [128, 512], F32)
            OTt = [sb(S2, "OTt%d" % i, [128, 512], BF16) for i in range(2)]
            psS = [[ps(S2, "psS%d_%d" % (m, i), [128, 512]) for i in range(2)] for m in range(2)]
            psO = [ps(S2, "psO%d" % m, [128, 512]) for m in range(2)]
            psZ = [ps(S2, "psZ%d" % m, [128, 512]) for m in range(2)]

            for W, src in ((WQ, wq), (WK, wk), (WV, wv)):
                for k in range(8):
                    P.dma("pool", lambda e, W=W, src=src, k=k: e.dma_start(
                        out=W[:, k, :], in_=src[k * 128:(k + 1) * 128, :]), w=[W])
            for m in range(2):
                P.dma("sp", lambda e, m=m: e.dma_start(out=QA[m][64:68, :], in_=qaug), w=[QA[m]])
            for i in range(4):
                P.dma("sp", lambda e, i=i: e.dma_start(out=LQ[i][:, :], in_=lam[i, :].partition_broadcast(128)),
                      w=[LQ[i]])
            P.dma("sp", lambda e: e.dma_start(out=GSC[:, :], in_=sgin), w=[GSC])
            P.op("dve", lambda e: e.memset(EPS[:, :], LN_EPS), w=[EPS])
            for j in range(2):
                P.op("dve", lambda e, j=j: e.tensor_tensor(LQ[2 * j][:, :], LQ[2 * j][:, :], LQ[2 * j + 1][:, :], ALU.mult),
                     r=[LQ[2 * j], LQ[2 * j + 1]], w=[LQ[2 * j]])
                P.op("dve", lambda e, j=j: e.reduce_sum(LS[:, j:j + 1], LQ[2 * j][:, :], AX.X), r=[LQ[2 * j]], w=[LS])
            P.op("act", lambda e: e.activation(LS[:, 2:4], LS[:, 0:2], AF.Exp), r=[LS], w=[LS])
            P.op("dve", lambda e: e.tensor_tensor(NLAM[:, :], LS[:, 3:4], LS[:, 2:3], ALU.subtract), r=[LS], w=[NLAM])
            P.op("dve", lambda e: e.tensor_scalar(NLAM[:, :], NLAM[:, :], -lambda_init, None, ALU.add),
                 r=[NLAM], w=[NLAM])
            P.op("dve", lambda e: e.tensor_scalar(GSC[:, :], GSC[:, :], 1.0 - lambda_init, None, ALU.mult),
                 r=[GSC], w=[GSC])

            npt = 0
            for h in range(4):
                for m in range(2):
                    P.dma("sp", lambda e, m=m, h=h: e.dma_start(out=KA[m][64:68, :], in_=kaug[h, :, :]), w=[KA[m]])
                for s in range(NCHK):
                    xc = XTc[s % 2]
                    P.dma("sp", lambda e, xc=xc, s=s: e.dma_start(out=xc[:, :, :], in_=XTv[:, :, s * 512:(s + 1) * 512]),
                          r=["XTd"], w=[xc])
                    for (W, dst, sc) in ((WQ, QA, 0.125), (WK, KA, 1.0)):
                        for m in range(2):
                            pp = psS[m][s % 2]
                            c0 = h * 128 + m * 64
                            for k in range(8):
                                P.op("pe", lambda e, pp=pp, W=W, k=k, c0=c0, xc=xc: e.matmul(
                                    pp[0:64, :], W[:, k, c0:c0 + 64], xc[:, k, :], start=(k == 0), stop=(k == 7)),
                                    r=[W, xc], w=[pp])
                            P.op("act", lambda e, pp=pp, dst=dst, m=m, s=s, sc=sc: e.activation(
                                dst[m][0:64, s * 512:(s + 1) * 512], pp[0:64, :], AF.Copy, scale=sc),
                                r=[pp], w=[dst[m]])
                    for t in range(4):
                        pp = psO[t % 2]
                        for k in range(8):
                            P.op("pe", lambda e, pp=pp, k=k, t=t, xc=xc, h=h: e.matmul(
                                pp[:, 0:128], xc[:, k, t * 128:(t + 1) * 128], WV[:, k, h * 128:(h + 1) * 128],
                                start=(k == 0), stop=(k == 7)), r=[WV, xc], w=[pp])
                        P.op("dve", lambda e, pp=pp, s=s, t=t: e.tensor_copy(V[:, s * 4 + t, :], pp[:, 0:128]),
                             r=[pp], w=[V])
                for i in range(NCHK):
                    nkb = 4 * i + 4
                    for kb in range(nkb):
                        jl = kb - 4 * i
                        c0 = 128 * jl if jl > 0 else 0
                        pts = []
                        for m in range(2):
                            pS = psS[m][npt % 2]
                            pt = PT[m][npt % 2]
                            pts.append(pt)
                            P.op("pe", lambda e, pS=pS, m=m, kb=kb, i=i, c0=c0: e.matmul(
                                pS[:, c0:512], KA[m][0:68, kb * 128:(kb + 1) * 128],
                                QA[m][0:68, i * 512 + c0:(i + 1) * 512], start=True, stop=True),
                                r=[KA[m], QA[m]], w=[pS])
                            P.op("act", lambda e, pS=pS, pt=pt, c0=c0: e.activation(pt[:, c0:512], pS[:, c0:512], AF.Exp),
                                 r=[pS], w=[pt])
                            if jl >= 0:
                                P.op("dve", lambda e, pt=pt, c0=c0: e.tensor_tensor(
                                    pt[:, c0:c0 + 128], pt[:, c0:c0 + 128], TRB, ALU.mult), r=[pt, CSTB], w=[pt])
                        npt += 1
                        for m in range(2):
                            pt = pts[m]
                            P.op("pe", lambda e, m=m, pt=pt, kb=kb, c0=c0, nkb=nkb: e.matmul(
                                psO[m][:, c0:512], V[:, kb, :], pt[:, c0:512], start=(kb == 0), stop=(kb == nkb - 1)),
                                r=[V, pt], w=[psO[m]])
                            P.op("pe", lambda e, m=m, pt=pt, kb=kb, c0=c0, nkb=nkb: e.matmul(
                                psZ[m][:, c0:512], ONB, pt[:, c0:512], start=(kb == 0), stop=(kb == nkb - 1)),
                                r=[CSTB, pt], w=[psZ[m]])
                    P.op("dve", lambda e: e.reciprocal(R1[:, :], psZ[0][:, :]), r=[psZ[0]], w=[R1])
                    P.op("dve", lambda e: e.reciprocal(R2[:, :], psZ[1][:, :]), r=[psZ[1]], w=[R2])
                    P.op("dve", lambda e: e.tensor_tensor(OA[:, :], psO[0][:, :], R1[:, :], ALU.mult), r=[psO[0], R1], w=[OA])
                    P.op("dve", lambda e: e.tensor_tensor(OB[:, :], psO[1][:, :], R2[:, :], ALU.mult), r=[psO[1], R2], w=[OB])
                    P.op("dve", lambda e: e.scalar_tensor_tensor(OA[:, :], OB[:, :], NLAM[:, 0:1], OA[:, :], ALU.mult, ALU.add),
                         r=[OA, OB, NLAM], w=[OA])
                    P.op("dve", lambda e: e.tensor_tensor(SQ[:, :], OA[:, :], OA[:, :], ALU.mult), r=[OA], w=[SQ])
                    pR = psS[0][npt % 2]
                    P.op("pe", lambda e, pR=pR: e.matmul(pR[:, :], ONF, SQ[:, :], start=True, stop=True), r=[CST, SQ], w=[pR])
                    P.op("act", lambda e, pR=pR: e.activation(R1[:, :], pR[:, :], AF.Sqrt, bias=EPS[:, 0:1], scale=1.0 / 128),
                         r=[pR, EPS], w=[R1])
                    P.op("dve", lambda e: e.reciprocal(R1[:, :], R1[:, :]), r=[R1], w=[R1])
                    P.op("dve", lambda e: e.tensor_tensor(OA[:, :], OA[:, :], R1[:, :], ALU.mult), r=[OA, R1], w=[OA])
                    ot = OTt[i % 2]
                    P.op("dve", lambda e, ot=ot: e.tensor_scalar(ot[:, :], OA[:, :], GSC[:, 0:1], None, ALU.mult),
                         r=[OA, GSC], w=[ot])
                    P.dma("sp", lambda e, ot=ot, h=h, i=i: e.dma_start(out=oT[h, :, i * 512:(i + 1) * 512], in_=ot[:, :]),
                          r=[ot], w=["oTd"])
            P.barrier()
            P.flush()
    return nc


def _ret_consts(heads):
    C = 128
    out = np.zeros((len(heads), 128, 128 + 512 + 2), np.float32)
    pos = np.arange(C, dtype=np.float64)
    for i, h in enumerate(heads):
        lg = math.log1p(-2.0 ** (-5.0 - h))
        diff = pos[None, :] - pos[:, None]
        dmT = np.where(diff >= 0, np.exp(lg * np.maximum(diff, 0.0)), 0.0)
        out[i, :, 0:128] = dmT
        qd = np.exp(lg * (pos + 1.0))
        out[i, :, 128:640] = np.tile(qd, 4)[None, :]
        out[i, :, 640] = np.exp(lg * (C - 1.0 - pos)) / 16.0
        out[i, :, 641] = math.exp(lg * C)
    return out


def build_phaseA_ret(dbg_nh=2, dbg_nchk=None, dbg_stage=9):
    nc = bass.Bass("TRN2", target_bir_lowering=False)

    def din(name, shape, dt):
        return nc.dram_tensor(name, shape, dt, kind="ExternalInput").ap()

    x = din("x", [SEQ, D], F32)
    wq = din("wq", [D, 512], F32)
    wk = din("wk", [D, 512], F32)
    wv = din("wv", [D, 1024], F32)
    wg = din("wg", [D, 1024], F32)
    rdec = din("rdec", [2, 128, 642], F32)
    cst = din("cstA", [128, 384], F32)
    oT = nc.dram_tensor("oT", [8, 128, SEQ], BF16, kind="ExternalOutput").ap()
    XT = nc.dram_tensor("XT", [8, 128, SEQ], BF16, kind="Internal").ap()
    XTv = XT.rearrange("k p t -> p k t")
    oTv = oT.rearrange("k p t -> p k t")
    NCHK = SEQ // 512

    with ExitStack() as S0:
        P = Prog(nc, S0)

        def sb(stack, name, shape, dt):
            return stack.enter_context(nc.sbuf_tensor(name, shape, dt))

        def ps(stack, name, shape, dt=F32):
            return stack.enter_context(nc.psum_tensor(name, shape, dt))

        CSTB = sb(S0, "CSTB", [128, 384], BF16)
        P.dma("pool", lambda e: e.dma_start(out=CSTB[:, :], in_=cst), w=[CSTB])
        IDB = CSTB[:, 0:128]

        with ExitStack() as S1:
            XB = [sb(S1, "XB%d" % i, [128, D], BF16) for i in range(2)]
            XTg = [sb(S1, "XTg%d" % i, [128, 8, 512], BF16) for i in range(2)]
            psX = [ps(S1, "psX%d" % i, [128, D], BF16) for i in range(2)]
            for s in range(NCHK if dbg_nchk is None else dbg_nchk):
                g = XTg[s % 2]
                for t in range(4):
                    T = s * 4 + t
                    xb = XB[T % 2]
                    px = psX[T % 2]
                    P.dma("pool", lambda e, xb=xb, T=T: e.dma_start(out=xb[:, :], in_=x[T * 128:(T + 1) * 128, :]),
                          w=[xb])
                    for k in range(8):
                        P.op("pe", lambda e, px=px, xb=xb, k=k: e.transpose(
                            px[:, k * 128:(k + 1) * 128], xb[:, k * 128:(k + 1) * 128], IDB), r=[xb, CSTB], w=[px])
                    if t % 2 == 0:
                        P.op("act", lambda e, g=g, px=px, t=t: e.copy(
                            g[:, :, t * 128:(t + 1) * 128], px[:, :].rearrange("p (k t) -> p k t", k=8)),
                            r=[px], w=[g])
                    else:
                        P.op("dve", lambda e, g=g, px=px, t=t: e.tensor_copy(
                            g[:, :, t * 128:(t + 1) * 128], px[:, :].rearrange("p (k t) -> p k t", k=8)),
                            r=[px], w=[g])
                P.dma("sp", lambda e, g=g, s=s: e.dma_start(out=XTv[:, :, s * 512:(s + 1) * 512], in_=g[:, :, :]),
                      r=[g], w=["XTd"])
            P.barrier()
            P.flush()

        with ExitStack() as S2:
            WQ = sb(S2, "WQ", [128, 8, 512], BF16)
            WK = sb(S2, "WK", [128, 8, 512], BF16)
            WV = sb(S2, "WV", [128, 8, 1024], BF16)
            WG = sb(S2, "WG", [128, 8, 1024], BF16)
            RD = [sb(S2, "RD%d" % i, [128, 642], F32) for i in range(2)]
            XTc = [sb(S2, "XTc%d" % i, [128, 8, 512], BF16) for i in range(2)]
            QT = [sb(S2, "QT%d" % i, [128, 512], BF16) for i in range(2)]
            QTd = [sb(S2, "QTd%d" % i, [128, 512], BF16) for i in range(2)]
            KT = [sb(S2, "KT%d" % i, [128, 512], BF16) for i in range(2)]
            Kd = [sb(S2, "Kd%d" % i, [128, 256], BF16) for i in range(4)]
            Vt = [sb(S2, "Vt%d" % i, [128, 512], BF16) for i in range(4)]
            SG = [sb(S2, "SG%d" % i, [128, 512], F32) for i in range(4)]
            ST = [sb(S2, "ST%d" % i, [128, 512], F32) for i in range(2)]
            STb = [sb(S2, "STb%d" % i, [128, 512], BF16) for i in range(2)]
            SCT = [sb(S2, "SCT%d" % i, [128, 128], BF16) for i in range(2)]
            ON = [sb(S2, "ON%d" % i, [128, 512], F32) for i in range(2)]
            OG = [sb(S2, "OG%d" % i, [128, 512], BF16) for i in range(2)]
            OTg = [sb(S2, "OTg%d" % i, [128, 4, 512], BF16) for i in range(2)]
            st = sb(S2, "st", [128, 6], F32)
            mv = sb(S2, "mv", [128, 2], F32)
            rstd = sb(S2, "rstd", [128, 1], F32)
            EPS = sb(S2, "EPS", [128, 1], F32)
            psP = [ps(S2, "psP%d" % i, [128, 512]) for i in range(2)]
            psS = ps(S2, "psS", [128, 512])
            psO = ps(S2, "psO", [128, 512])
            psU = [ps(S2, "psU%d" % i, [128, 512]) for i in range(2)]
            psTr = ps(S2, "psTr", [128, 512], BF16)

            for W, src in ((WQ, wq), (WK, wk), (WV, wv), (WG, wg)):
                for k in range(8):
                    P.dma("pool", lambda e, W=W, src=src, k=k: e.dma_start(
                        out=W[:, k, :], in_=src[k * 128:(k + 1) * 128, :]), w=[W])
            for h in range(2):
                P.dma("sp", lambda e, h=h: e.dma_start(out=RD[h][:, :], in_=rdec[h, :, :]), w=[RD[h]])
            P.op("dve", lambda e: e.memset(EPS[:, :], LN_EPS), w=[EPS])
            npp = 0
            for h in range(dbg_nh):
                DMT = RD[h][:, 0:128]
                QD4 = RD[h][:, 128:640]
                KDC = RD[h][:, 640:641]
                CDC = RD[h][:, 641:642]
                for c2 in range(2):
                    P.op("dve", lambda e, c2=c2: e.memset(ST[c2][:, :], 0.0), w=[ST[c2]])
                    if 'mset' not in DBG_SKIP:
                        P.op("dve", lambda e, c2=c2: e.memset(STb[c2][:, :], 0.0), w=[STb[c2]])
                for s in range(NCHK if dbg_nchk is None else dbg_nchk):
                    xc = XTc[s % 2]
                    P.dma("sp", lambda e, xc=xc, s=s: e.dma_start(out=xc[:, :, :], in_=XTv[:, :, s * 512:(s + 1) * 512]),
                          r=["XTd"], w=[xc])
                    for c2 in range(2):
                        c0 = h * 256 + c2 * 128
                        pp = psP[npp % 2]; npp += 1
                        for k in range(8):
                            P.op("pe", lambda e, pp=pp, k=k, c0=c0, xc=xc: e.matmul(
                                pp[:, :], WQ[:, k, c0:c0 + 128], xc[:, k, :], start=(k == 0), stop=(k == 7)),
                                r=[WQ, xc], w=[pp])
                        P.op("dve", lambda e, pp=pp, c2=c2: e.tensor_copy(QT[c2][:, :], pp[:, :]), r=[pp], w=[QT[c2]])
                        if 'qtd' not in DBG_SKIP:
                            P.op("dve", lambda e, pp=pp, c2=c2, QD4=QD4: e.tensor_tensor(QTd[c2][:, :], pp[:, :], QD4, ALU.mult),
                                 r=[pp, RD[h]], w=[QTd[c2]])
                        pp = psP[npp % 2]; npp += 1
                        for k in range(8):
                            P.op("pe", lambda e, pp=pp, k=k, c0=c0, xc=xc: e.matmul(
                                pp[:, :], WK[:, k, c0:c0 + 128], xc[:, k, :], start=(k == 0), stop=(k == 7)),
                                r=[WK, xc], w=[pp])
                        if 'kt' not in DBG_SKIP:
                            P.op("act", lambda e, pp=pp, c2=c2: e.activation(KT[c2][:, :], pp[:, :], AF.Copy, scale=1.0 / 16),
                                 r=[pp], w=[KT[c2]])
                    for t in range(4):
                        pp = psP[npp % 2]; npp += 1
                        for k in range(8):
                            P.op("pe", lambda e, pp=pp, k=k, t=t, xc=xc, h=h: e.matmul(
                                pp[:, 0:256], xc[:, k, t * 128:(t + 1) * 128], WK[:, k, h * 256:(h + 1) * 256],
                                start=(k == 0), stop=(k == 7)), r=[WK, xc], w=[pp])
                        if 'kd' not in DBG_SKIP:
                            P.op("dve", lambda e, pp=pp, t=t, KDC=KDC: e.tensor_scalar(
                                Kd[t][:, :], pp[:, 0:256], KDC, None, ALU.mult), r=[pp, RD[h]], w=[Kd[t]])
                        pp = psP[npp % 2]; npp += 1
                        for k in range(8):
                            P.op("pe", lambda e, pp=pp, k=k, t=t, xc=xc, h=h: e.matmul(
                                pp[:, :], xc[:, k, t * 128:(t + 1) * 128], WV[:, k, h * 512:(h + 1) * 512],
                                start=(k == 0), stop=(k == 7)), r=[WV, xc], w=[pp])
                        P.op("act", lambda e, pp=pp, t=t: e.copy(Vt[t][:, :], pp[:, :]), r=[pp], w=[Vt[t]])
                        pp = psP[npp % 2]; npp += 1
                        for k in range(8):
                            P.op("pe", lambda e, pp=pp, k=k, t=t, xc=xc, h=h: e.matmul(
                                pp[:, :], xc[:, k, t * 128:(t + 1) * 128], WG[:, k, h * 512:(h + 1) * 512],
                                start=(k == 0), stop=(k == 7)), r=[WG, xc], w=[pp])
                        if 'sg' not in DBG_SKIP:
                            P.op("act", lambda e, pp=pp, t=t: e.activation(SG[t][:, :], pp[:, :], AF.Sigmoid), r=[pp], w=[SG[t]])
                        if 'sg' not in DBG_SKIP:
                            P.op("dve", lambda e, pp=pp, t=t: e.tensor_tensor(SG[t][:, :], SG[t][:, :], pp[:, :], ALU.mult),
                                 r=[pp, SG[t]], w=[SG[t]])
                    og4 = OTg[s % 2]
                    for t in range(4 if dbg_stage >= 2 else 0):
                        tt = slice(t * 128, (t + 1) * 128)
                        sct = SCT[t % 2]
                        for c2 in range(2):
                            P.op("pe", lambda e, c2=c2, tt=tt: e.matmul(
                                psS[:, 0:128], KT[c2][:, tt], QT[c2][:, tt], start=(c2 == 0), stop=(c2 == 1)),
                                r=[KT[c2], QT[c2]], w=[psS])
                        P.op("dve", lambda e, sct=sct, DMT=DMT: e.tensor_tensor(sct[:, :], psS[:, 0:128], DMT, ALU.mult),
                             r=[psS, RD[h]], w=[sct])
                        P.op("pe", lambda e, sct=sct, t=t: e.matmul(psO[:, :], sct[:, :], Vt[t][:, :], start=True, stop=False),
                             r=[sct, Vt[t]], w=[psO])
                        for c2 in range(2):
                            P.op("pe", lambda e, c2=c2, tt=tt: e.matmul(
                                psO[:, :], QTd[c2][:, tt], STb[c2][:, :], start=False, stop=(c2 == 1)),
                                r=[QTd[c2], STb[c2]], w=[psO])
                        for c2 in range(2 if dbg_stage >= 3 else 0):
                            P.op("pe", lambda e, c2=c2, t=t: e.matmul(
                                psU[c2][:, :], Kd[t][:, c2 * 128:(c2 + 1) * 128], Vt[t][:, :], start=True, stop=True),
                                r=[Kd[t], Vt[t]], w=[psU[c2]])
                            P.op("dve", lambda e, c2=c2, CDC=CDC: e.scalar_tensor_tensor(
                                ST[c2][:, :], ST[c2][:, :], CDC, psU[c2][:, :], ALU.mult, ALU.add),
                                r=[ST[c2], psU[c2], RD[h]], w=[ST[c2]])
                            P.op("act", lambda e, c2=c2: e.copy(STb[c2][:, :], ST[c2][:, :]), r=[ST[c2]], w=[STb[c2]])
                        on = ON[t % 2]
                        og = OG[t % 2]
                        if dbg_stage < 4:
                            continue
                        P.op("dve", lambda e: e.bn_stats(st[:, :], psO[:, :]), r=[psO], w=[st])
                        P.op("dve", lambda e: e.bn_aggr(mv[:, :], st[:, :]), r=[st], w=[mv])
                        P.op("act", lambda e: e.activation(rstd[:, :], mv[:, 1:2], AF.Sqrt, bias=EPS[:, 0:1], scale=1.0),
                             r=[mv, EPS], w=[rstd])
                        P.op("dve", lambda e: e.reciprocal(rstd[:, :], rstd[:, :]), r=[rstd], w=[rstd])
                        P.op("dve", lambda e, on=on: e.tensor_scalar(on[:, :], psO[:, :], mv[:, 0:1], rstd[:, 0:1],
                                                                     ALU.subtract, ALU.mult), r=[psO, mv, rstd], w=[on])
                        P.op("dve", lambda e, on=on, og=og, t=t: e.tensor_tensor(og[:, :], on[:, :], SG[t][:, :], ALU.mult),
                             r=[on, SG[t]], w=[og])
                        if dbg_stage < 5:
                            continue
                        for j in range(4):
                            P.op("pe", lambda e, og=og, j=j: e.transpose(
                                psTr[:, j * 128:(j + 1) * 128], og[:, j * 128:(j + 1) * 128], IDB), r=[og, CSTB], w=[psTr])
                        P.op("act", lambda e, og4=og4, tt=tt: e.copy(
                            og4[:, :, tt], psTr[:, :].rearrange("p (j t) -> p j t", j=4)), r=[psTr], w=[og4])
                    if dbg_stage >= 6:
                        P.dma("sp", lambda e, og4=og4, h=h, s=s: e.dma_start(
                            out=oTv[:, h * 4:(h + 1) * 4, s * 512:(s + 1) * 512], in_=og4[:, :, :]), r=[og4], w=["oTd"])
            P.barrier()
            P.flush()
    return nc


_PROGS = {}


def _prog(key, builder):
    if key not in _PROGS:
        _PROGS[key] = builder()
    return _PROGS[key]


def _run(nc, ims):
    return run_bass_kernel_spmd(nc, ims, core_ids=list(range(NCORE))).results


def _mixer_inputs(inp, i, X):
    j = i // 2
    cstA = _consts_A()
    ims = []
    for c in range(NCORE):
        b, hg = c // 2, c % 2
        xb = np.ascontiguousarray(X[b])
        if i % 2 == 0:
            w_in = inp["da_w_in"][j]
            heads = list(range(hg * 4, hg * 4 + 4))
            qaug, kaug = _alibi_aug(heads)
            ims.append({
                "x": xb,
                "wq": np.ascontiguousarray(w_in[:, hg * 512:(hg + 1) * 512]),
                "wk": np.ascontiguousarray(w_in[:, 1024 + hg * 512:1024 + (hg + 1) * 512]),
                "wv": np.ascontiguousarray(w_in[:, 2048 + hg * 512:2048 + (hg + 1) * 512]),
                "lam": np.stack([inp["da_lam_q1"][j], inp["da_lam_k1"][j], inp["da_lam_q2"][j], inp["da_lam_k2"][j]]),
                "sg": np.ascontiguousarray(inp["da_subln_g"][j].reshape(128, 1)),
                "qaug": qaug, "kaug": kaug, "cstA": cstA})
        else:
            w_in = inp["ret_w_in"][j]
            ims.append({
                "x": xb,
                "wq": np.ascontiguousarray(w_in[:, hg * 512:(hg + 1) * 512]),
                "wk": np.ascontiguousarray(w_in[:, 1024 + hg * 512:1024 + (hg + 1) * 512]),
                "wv": np.ascontiguousarray(w_in[:, 2048 + hg * 1024:2048 + (hg + 1) * 1024]),
                "wg": np.ascontiguousarray(w_in[:, 4096 + hg * 1024:4096 + (hg + 1) * 1024]),
                "rdec": _ret_consts([2 * hg, 2 * hg + 1]), "cstA": cstA})
    return ims


def _ffn_inputs(inp, i, X, oTs, cstB):
    j = i // 2
    w_out = inp["da_w_out"][j] if i % 2 == 0 else inp["ret_w_out"][j]
    lnp = np.stack([inp["ln_mix_g"][i], inp["ln_mix_b"][i], inp["ln_ffn_g"][i], inp["ln_ffn_b"][i]])
    bgu = np.ascontiguousarray(inp["moe_b_gate_up"][i].reshape(NE, 16, 128).transpose(2, 0, 1).reshape(128, NE * 16))
    ims = []
    wgu_full = np.ascontiguousarray(inp["moe_w_gate_up"][i])
    wd_full = np.ascontiguousarray(inp["moe_w_down"][i])
    for c in range(NCORE):
        b, half = c // 2, c % 2
        sl = slice(half * NT, (half + 1) * NT)
        oT = np.ascontiguousarray(np.concatenate([oTs[2 * b + hg][:, :, sl] for hg in range(2)], axis=0))
        ims.append({
            "xres": np.ascontiguousarray(X[b, sl]), "oT": oT, "wout": np.ascontiguousarray(w_out), "lnp": lnp,
            "wr": np.ascontiguousarray(inp["moe_w_router"][i]), "br": np.ascontiguousarray(inp["moe_b_router"][i][None, :]),
            "wgu": wgu_full, "bgu": bgu,
            "wd": wd_full,
            "bd": np.ascontiguousarray(inp["moe_b_down"][i]), "cst": cstB})
    return ims


def kernel(**inp):
    inp = {k: np.asarray(v) for k, v in inp.items()}
    X = np.array(inp["x"], dtype=np.float32, copy=True)
    for i in range(4):
        if i % 2 == 0:
            ncA = _prog(("da", i), lambda: build_phaseA_da(i))
            KC = 8
        else:
            ncA = _prog(("ret",), build_phaseA_ret)
            KC = 16
        resA = _run(ncA, _mixer_inputs(inp, i, X))
        oTs = [r["oT"] for r in resA]
        ncB, cstB = _prog(("B", KC), lambda: build_phaseB(KC))
        resB = _run(ncB, _ffn_inputs(inp, i, X, oTs, cstB))
        Xn = np.empty_like(X)
        for c in range(NCORE):
            b, half = c // 2, c % 2
            Xn[b, half * NT:(half + 1) * NT] = resB[c]["xout"]
        X = Xn
    return X
```

```python
import math
from contextlib import ExitStack

import numpy as np
import ml_dtypes

import concourse.bass as bass
import concourse.mybir as mybir
from concourse.bass_utils import run_bass_kernel_spmd

F32 = mybir.dt.float32
BF16 = mybir.dt.bfloat16
I32 = mybir.dt.int32
AF = mybir.ActivationFunctionType
ALU = mybir.AluOpType
AX = mybir.AxisListType

D = 1024
NCORE = 8
SEQ = 8192
BATCH = 4
NT = 4096
NTILE = NT // 128
NE = 32
CAP = 640
NCH = CAP // 128
ALPHA = (2.0 * 4) ** 0.25
LN_EPS = 1e-5
SAME_ENG_SYNC = True
DBG_SKIP = set()


class Prog:
    ENG = ("pe", "act", "dve", "pool", "sp")

    def __init__(self, nc, stack, nch=None):
        self.nc = nc
        nch = nch or {"sp": 16, "act": 4, "pool": 16, "cc": 8}
        self.ccsems = set()
        self.sem = {e: stack.enter_context(nc.semaphore("s_" + e)) for e in self.ENG}
        self.cnt = {e: 0 for e in self.ENG}
        self.ch = {q: [stack.enter_context(nc.semaphore("c_%s%d" % (q, i))) for i in range(n)]
                   for q, n in nch.items()}
        self.chcnt = {q: [0] * n for q, n in nch.items()}
        self.chnext = {q: 0 for q in nch}
        self.semid = {}
        for e in self.ENG:
            self.semid[id(self.sem[e])] = ("E", e)
        self.ops = {e: [] for e in self.ENG}
        self.waited = {e: {} for e in self.ENG}
        self.last_w = {}
        self.readers = {}
        self.nops = 0

    @staticmethod
    def _key(k):
        if isinstance(k, (str, tuple)):
            return k
        return k.name

    def _record(self, eng, fn, r, w, tok, extra):
        deps = {}

        def add(t):
            s, v = t
            if deps.get(id(s), (None, 0))[1] < v:
                deps[id(s)] = (s, v)

        for k in r:
            k = self._key(k)
            if k in self.last_w:
                add(self.last_w[k])
            if isinstance(k, str) and k.startswith("ps"):
                for t in self.readers.get(k, {}).values():
                    add(t)
        for k in w:
            k = self._key(k)
            if k in self.last_w:
                add(self.last_w[k])
            for t in self.readers.get(k, {}).values():
                add(t)
        for t in extra:
            add(t)
        waits = []
        own = self.sem[eng]
        for s, v in deps.values():
            if s is own and (eng == "pe" or not SAME_ENG_SYNC):
                continue
            if self.waited[eng].get(id(s), 0) >= v:
                continue
            self.waited[eng][id(s)] = v
            waits.append((s, v))
        self.ops[eng].append((waits, fn, tok))
        self.nops += 1
        for k in w:
            k = self._key(k)
            self.last_w[k] = tok
            self.readers[k] = {}
        for k in r:
            k = self._key(k)
            d = self.readers.setdefault(k, {})
            s, v = tok
            if d.get(id(s), (None, 0))[1] < v:
                d[id(s)] = tok
        return tok

    def op(self, eng, fn, r=(), w=()):
        self.cnt[eng] += 1
        tok = (self.sem[eng], self.cnt[eng])
        return self._record(eng, fn, r, w, (tok[0], tok[1]), ())

    def dma(self, q, fn, r=(), w=()):
        i = self.chnext[q]
        self.chnext[q] = (i + 1) % len(self.ch[q])
        s = self.ch[q][i]
        extra = []
        if self.chcnt[q][i] > 0:
            extra.append((s, 16 * self.chcnt[q][i]))
        self.chcnt[q][i] += 1
        tok = (s, 16 * self.chcnt[q][i])
        self.cnt
        return self._record(q, fn, r, w, tok, extra)

    def cc(self, fn, r=(), w=()):
        if "cc" not in self.ch:
            raise RuntimeError("no cc channels")
        i = self.chnext["cc"]
        self.chnext["cc"] = (i + 1) % len(self.ch["cc"])
        s = self.ch["cc"][i]
        extra = []
        if self.chcnt["cc"][i] > 0:
            extra.append((s, self.chcnt["cc"][i]))
        self.chcnt["cc"][i] += 1
        tok = (s, self.chcnt["cc"][i])
        self.ccsems.add(id(s))
        return self._record("pool", fn, r, w, tok, extra)

    def all_tokens(self):
        toks = [(self.sem[e], self.cnt[e]) for e in self.ENG if self.cnt[e] > 0]
        for q in self.ch:
            for s, c in zip(self.ch[q], self.chcnt[q]):
                if c > 0:
                    toks.append((s, c if q == "cc" else 16 * c))
        return toks

    def barrier(self):
        toks = self.all_tokens()
        for e in self.ENG:
            waits = []
            for s, v in toks:
                if s is self.sem[e]:
                    continue
                if self.waited[e].get(id(s), 0) >= v:
                    continue
                self.waited[e][id(s)] = v
                waits.append((s, v))
            if waits:
                self.ops[e].append((waits, None, None))
        self.last_w = {}
        self.readers = {}

    def check(self, ops):
        if not hasattr(self, "_simval"):
            self._simval = {}
        val = self._simval
        pc = {e: 0 for e in self.ENG}
        progress = True
        while progress:
            progress = False
            for e in self.ENG:
                while pc[e] < len(ops[e]):
                    waits, fn, tok = ops[e][pc[e]]
                    if any(val.get(id(s), 0) < v for s, v in waits):
                        break
                    if tok is not None:
                        s, v = tok
                        inc = 1 if (id(s) in self.ccsems or any(s is x for x in self.sem.values())) else 16
                        val[id(s)] = val.get(id(s), 0) + inc
                        assert val[id(s)] == v, ("token mismatch", e, pc[e], val[id(s)], v)
                    pc[e] += 1
                    progress = True
        stuck = {e: (pc[e], len(ops[e])) for e in self.ENG if pc[e] < len(ops[e])}
        assert not stuck, ("DEADLOCK", stuck)

    def flush(self):
        nc = self.nc
        ops = self.ops
        self.ops = {e: [] for e in self.ENG}
        self.check(ops)

        def emit(eng_name, e):
            for waits, fn, tok in ops[eng_name]:
                for s, v in waits:
                    e.wait_ge(s, v)
                if fn is None:
                    continue
                ins = fn(e)
                s, v = tok
                if id(s) in self.ccsems:
                    ins.then_inc(s, 1)
                elif eng_name in self.ch and s is not self.sem[eng_name]:
                    ins.then_inc(s, 16)
                else:
                    ins.then_inc(s, 1)

        with nc.Block() as block:
            @block.tensor
            def _(e):
                emit("pe", e)

            @block.scalar
            def _(e):
                emit("act", e)

            @block.vector
            def _(e):
                emit("dve", e)

            @block.gpsimd
            def _(e):
                emit("pool", e)

            @block.sync
            def _(e):
                emit("sp", e)


def _bf16(a):
    return np.asarray(a).astype(ml_dtypes.bfloat16)


def _consts_B():
    c = {}
    c["ident"] = np.eye(128, dtype=np.float32)
    c["utri"] = np.triu(np.ones((128, 128), np.float32), 1)
    c["ones"] = np.ones((128, 128), np.float32)
    c["iota"] = np.tile(np.arange(CAP, dtype=np.float32)[None, :], (128, 1))
    ecap = np.tile((np.arange(NE, dtype=np.float32) * CAP)[None, None, :], (128, NTILE, 1))
    c["ecap"] = ecap.reshape(128, NTILE * NE)
    tp = np.zeros((128, NTILE, 2), np.float32)
    tp[:, :, 0] = np.arange(NTILE)[None, :]
    tp[:, :, 1] = np.arange(128)[:, None]
    c["tp"] = tp.reshape(128, NTILE * 2)
    names = ["ident", "utri", "ones", "iota", "ecap", "tp"]
    offs = {}
    o = 0
    for n in names:
        offs[n] = (o, c[n].shape[1])
        o += c[n].shape[1]
    return np.concatenate([c[n] for n in names], axis=1), offs


def _layer_norm(P, R, OUT, G, Bt, T):
    st, mv, rstd = T["st"], T["mv"], T["rstd"]
    for h in range(2):
        P.op("dve", lambda e, h=h: e.bn_stats(st[:, h * 6:(h + 1) * 6], R[:, h * 512:(h + 1) * 512]),
             r=[R], w=[st])
    P.op("dve", lambda e: e.bn_aggr(mv[:, :], st[:, :]), r=[st], w=[mv])
    P.op("act", lambda e: e.activation(rstd[:, :], mv[:, 1:2], AF.Sqrt, bias=T["eps"][:, 0:1], scale=1.0),
         r=[mv, T["eps"]], w=[rstd])
    P.op("dve", lambda e: e.reciprocal(rstd[:, :], rstd[:, :]), r=[rstd], w=[rstd])
    P.op("dve", lambda e: e.tensor_scalar(OUT[:, :], R[:, :], mv[:, 0:1], rstd[:, 0:1],
                                           ALU.subtract, ALU.mult), r=[R, mv, rstd], w=[OUT])
    P.op("dve", lambda e: e.tensor_tensor(OUT[:, :], OUT[:, :], G[:, :], ALU.mult), r=[OUT, G], w=[OUT])
    P.op("dve", lambda e: e.tensor_tensor(OUT[:, :], OUT[:, :], Bt[:, :], ALU.add), r=[OUT, Bt], w=[OUT])


def build_phaseB(KC, ag=False):
    nc = bass.Bass("TRN2", target_bir_lowering=False)
    cst_np, co = _consts_B()
    NCST = cst_np.shape[1]

    def din(name, shape, dt):
        return nc.dram_tensor(name, shape, dt, kind="ExternalInput").ap()

    xres = din("xres", [NT, D], F32)
    oT = din("oT", [KC, 128, NT], BF16)
    wout = din("wout", [KC * 128, D], F32)
    lnp = din("lnp", [4, D], F32)
    wr = din("wr", [D, NE], F32)
    br = din("br", [1, NE], F32)
    nw = 4 if ag else NE
    wgu = din("wgu", [nw, D, 2 * D], F32)
    bgu = din("bgu", [128, NE * 16], F32)
    wd = din("wd", [nw, D, D], F32)
    bd = din("bd", [NE, D], F32)
    cst = din("cst", [128, NCST], F32)
    xout = nc.dram_tensor("xout", [NT, D], F32, kind="ExternalOutput").ap()
    XM = nc.dram_tensor("XM", [NT, D], F32, kind="Internal").ap()
    Y = nc.dram_tensor("Y", [NE * CAP, D], F32, kind="Internal").ap()
    WB = nc.dram_tensor("WB", [4 * D, 3 * D] if ag else [128, 64], BF16)
    WA = nc.dram_tensor("WA", [NCORE * 4 * D, 3 * D] if ag else [128, 64], BF16)

    with ExitStack() as S0:
        P = Prog(nc, S0)

        def sb(stack, name, shape, dt):
            return stack.enter_context(nc.sbuf_tensor(name, shape, dt))

        def ps(stack, name, shape, dt=F32):
            return stack.enter_context(nc.psum_tensor(name, shape, dt))

        CST = sb(S0, "CST", [128, NCST], F32)
        CSTB = sb(S0, "CSTB", [128, 3 * 128], BF16)
        TPB = sb(S0, "TPB", [128, NTILE * 2], BF16)
        G1 = sb(S0, "G1", [128, D], F32)
        B1 = sb(S0, "B1", [128, D], F32)
        G2 = sb(S0, "G2", [128, D], F32)
        B2 = sb(S0, "B2", [128, D], F32)
        BRt = sb(S0, "BRt", [128, NE], F32)
        EPS = sb(S0, "EPS", [128, 1], F32)
        MASK = sb(S0, "MASK", [128, NTILE * NE], F32)
        GATE = sb(S0, "GATE", [128, NTILE * NE], F32)
        LOG = sb(S0, "LOG", [128, NTILE * NE], F32)
        MX8 = sb(S0, "MX8", [128, NTILE * 8], F32)
        POSM = sb(S0, "POSM", [128, NTILE * NE], F32)
        SLJI = sb(S0, "SLJI", [128, NTILE * 4], I32)
        GJ = sb(S0, "GJ", [128, NTILE * 4], F32)
        TOKI = sb(S0, "TOKI", [128, NE * NCH], I32)
        STGg = [sb(S0, "STGg%d" % i, [128, 2 * D] if ag else [128, 2], BF16) for i in range(2)]
        STGd = [sb(S0, "STGd%d" % i, [128, D] if ag else [128, 2], BF16) for i in range(2)]
        LT = {k: sb(S0, "ln_" + k, shp, F32) for k, shp in
              [("st", [128, 12]), ("mv", [128, 2]), ("rstd", [128, 1])]}
        LT["eps"] = EPS

        def cs(name, lo=0, hi=None):
            o, n = co[name]
            hi = n if hi is None else hi
            return CST[:, o + lo:o + hi]

        WBa = WB.ap()
        ns = 0
        for le in range(4 if ag else 0):
            for k in range(8):
                sg_, sd_ = STGg[ns % 2], STGd[ns % 2]
                ns += 1
                r0 = le * D + k * 128
                P.dma("pool", lambda e, sg_=sg_, le=le, k=k: e.dma_start(
                    out=sg_[:, :], in_=wgu[le, k * 128:(k + 1) * 128, :]), w=[sg_])
                P.dma("sp", lambda e, sg_=sg_, r0=r0: e.dma_start(out=WBa[r0:r0 + 128, 0:2 * D], in_=sg_[:, :]),
                      r=[sg_], w=["WBd"])
                P.dma("pool", lambda e, sd_=sd_, le=le, k=k: e.dma_start(
                    out=sd_[:, :], in_=wd[le, k * 128:(k + 1) * 128, :]), w=[sd_])
                P.dma("sp", lambda e, sd_=sd_, r0=r0: e.dma_start(out=WBa[r0:r0 + 128, 2 * D:3 * D], in_=sd_[:, :]),
                      r=[sd_], w=["WBd"])
        if ag:
            P.cc(lambda e: e.collective_compute(
                "AllGather", ALU.bypass, replica_groups=[list(range(NCORE))],
                ins=[WB.ap().opt()], outs=[WA.ap().opt()]), r=["WBd"], w=["WAd"])
            P.barrier()
            P.flush()
        P.dma("sp", lambda e: e.dma_start(out=CST[:, :], in_=cst), w=[CST])
        o_id = co["ident"][0]
        P.dma("pool", lambda e: e.dma_start(out=CSTB[:, :], in_=cst[:, o_id:o_id + 384]), w=[CSTB])
        o_tp = co["tp"][0]
        P.dma("pool", lambda e: e.dma_start(out=TPB[:, :], in_=cst[:, o_tp:o_tp + NTILE * 2]), w=[TPB])
        for i, t in enumerate([G1, B1, G2, B2]):
            P.dma("sp", lambda e, i=i, t=t: e.dma_start(out=t[:, :], in_=lnp[i, :].partition_broadcast(128)),
                  w=[t])
        P.dma("sp", lambda e: e.dma_start(out=BRt[:, :], in_=br[0, :].partition_broadcast(128)), w=[BRt])
        P.op("dve", lambda e: e.memset(EPS[:, :], LN_EPS), w=[EPS])
        IDF = cs("ident")
        IDB = CSTB[:, 0:128]
        UTB = CSTB[:, 128:256]
        ONB = CSTB[:, 256:384]

        with ExitStack() as S1:
            WOUT = sb(S1, "WOUT", [128, KC, D], BF16)
            WR = sb(S1, "WR", [128, 8, NE], F32)
            OTs = [sb(S1, "OT%d" % i, [128, KC, 512], BF16) for i in range(2)]
            XR = [sb(S1, "XR%d" % i, [128, D], F32) for i in range(2)]
            R = [sb(S1, "R%d" % i, [128, D], F32) for i in range(2)]
            XMt = [sb(S1, "XMt%d" % i, [128, D], F32) for i in range(2)]
            XMT = [sb(S1, "XMT%d" % i, [128, D], F32) for i in range(2)]
            Lt = sb(S1, "Lt", [128, NE], F32)
            EX = sb(S1, "EX", [128, NE], F32)
            SM = sb(S1, "SM", [128, 4], F32)
            psA = [ps(S1, "psA%d" % i, [128, 512]) for i in range(4)]
            psT = [ps(S1, "psT%d" % i, [128, D]) for i in range(1)]
            psL = ps(S1, "psL", [128, 512])

            for k in range(KC):
                P.dma("pool", lambda e, k=k: e.dma_start(out=WOUT[:, k, :], in_=wout[k * 128:(k + 1) * 128, :]),
                      w=[("WOUT", k)])
            P.dma("sp", lambda e: e.dma_start(out=WR[:, :, :], in_=wr.rearrange("(k p) n -> p k n", p=128)), w=[WR])
            oTv = oT.rearrange("k p t -> p k t")
            for s in range(NT // 512):
                OTb = OTs[s % 2]
                P.dma("sp", lambda e, s=s, OTb=OTb: e.dma_start(out=OTb[:, :, :], in_=oTv[:, :, s * 512:(s + 1) * 512]),
                      w=[OTb])
                for t in range(4):
                    T = s * 4 + t
                    b = T % 2
                    P.dma("sp", lambda e, T=T, b=b: e.dma_start(out=XR[b][:, :], in_=xres[T * 128:(T + 1) * 128, :]),
                          w=[XR[b]])
                    for h in range(2):
                        pa = psA[(T * 2 + h) % 4]
                        for k in range(KC):
                            P.op("pe", lambda e, pa=pa, OTb=OTb, k=k, t=t, h=h: e.matmul(
                                pa[:, :], OTb[:, k, t * 128:(t + 1) * 128], WOUT[:, k, h * 512:(h + 1) * 512],
                                start=(k == 0), stop=(k == KC - 1)),
                                r=[OTb, ("WOUT", k)], w=[pa])
                        P.op("dve", lambda e, pa=pa, b=b, h=h: e.scalar_tensor_tensor(
                            R[b][:, h * 512:(h + 1) * 512], XR[b][:, h * 512:(h + 1) * 512], ALPHA, pa[:, :],
                            ALU.mult, ALU.add), r=[pa, XR[b]], w=[R[b]])
                    _layer_norm(P, R[b], XMt[b], G1, B1, LT)
                    P.dma("sp", lambda e, T=T, b=b: e.dma_start(out=XM[T * 128:(T + 1) * 128, :], in_=XMt[b][:, :]),
                          r=[XMt[b]], w=["XMd"])
                    pT = psT[0]
                    for k in range(8):
                        P.op("pe", lambda e, pT=pT, b=b, k=k: e.transpose(
                            pT[:, k * 128:(k + 1) * 128], XMt[b][:, k * 128:(k + 1) * 128], IDF),
                            r=[XMt[b], CST], w=[pT])
                    P.op("act", lambda e, pT=pT, b=b: e.copy(XMT[b][:, :], pT[:, :]), r=[pT], w=[XMT[b]])
                    for k in range(8):
                        P.op("pe", lambda e, b=b, k=k: e.matmul(
                            psL[:, 0:NE], XMT[b][:, k * 128:(k + 1) * 128], WR[:, k, :],
                            start=(k == 0), stop=(k == 7)), r=[XMT[b], WR], w=[psL])
                    lg = LOG[:, T * NE:(T + 1) * NE]
                    mk = MASK[:, T * NE:(T + 1) * NE]
                    gt = GATE[:, T * NE:(T + 1) * NE]
                    m8 = MX8[:, T * 8:(T + 1) * 8]
                    P.op("dve", lambda e, lg=lg: e.tensor_tensor(lg, psL[:, 0:NE], BRt[:, :], ALU.add),
                         r=[psL, BRt], w=[LOG])
                    P.op("dve", lambda e, lg=lg, m8=m8: e.max(m8, lg), r=[LOG], w=[MX8])
                    P.op("dve", lambda e, lg=lg, m8=m8, mk=mk: e.tensor_scalar(
                        mk, lg, m8[:, 3:4], None, ALU.is_ge), r=[LOG, MX8], w=[MASK])
                    P.op("dve", lambda e, m8=m8: e.tensor_scalar(
                        SM[:, 0:1], m8[:, 0:1], -1.0, None, ALU.mult), r=[MX8], w=[SM])
                    P.op("act", lambda e, lg=lg: e.activation(EX[:, :], lg, AF.Exp, bias=SM[:, 0:1], scale=1.0),
                         r=[LOG, SM], w=[EX])
                    P.op("dve", lambda e, mk=mk: e.tensor_tensor(EX[:, :], EX[:, :], mk, ALU.mult),
                         r=[EX, MASK], w=[EX])
                    P.op("dve", lambda e: e.reduce_sum(SM[:, 1:2], EX[:, :], AX.X), r=[EX], w=[SM])
                    P.op("dve", lambda e: e.reciprocal(SM[:, 2:3], SM[:, 1:2]), r=[SM], w=[SM])
                    P.op("dve", lambda e, gt=gt: e.tensor_scalar(gt, EX[:, :], SM[:, 2:3], None, ALU.mult),
                         r=[EX, SM], w=[GATE])
            P.barrier()
            P.flush()

        with ExitStack() as S2:
            MASKB = sb(S2, "MASKB", [128, NTILE * NE], BF16)
            CNT = sb(S2, "CNT", [128, NTILE * NE], F32)
            OFF = sb(S2, "OFF", [128, NTILE * NE], F32)
            SL = sb(S2, "SL", [128, NTILE * NE], F32)
            SLJ = sb(S2, "SLJ", [128, NTILE * 4], F32)
            TMP = sb(S2, "TMP", [128, NE], F32)
            OH = [sb(S2, "OH%d" % i, [128, CAP], BF16) for i in range(2)]
            TOKF = sb(S2, "TOKF", [128, NE * NCH], F32)
            TK = [sb(S2, "TK%d" % i, [128, 2 * NCH], F32) for i in range(2)]
            psW = [ps(S2, "psW%d" % i, [128, 512]) for i in range(2)]
            psC = [ps(S2, "psC%d" % i, [128, 512]) for i in range(2)]
            psI = [ps(S2, "psI%d" % i, [128, 512]) for i in range(2)]
            ZB = sb(S2, "ZB", [128, 128], BF16)
            P.op("dve", lambda e: e.memset(ZB[:, :], 0.0), w=[ZB])
            P.op("dve", lambda e: e.tensor_copy(MASKB[:, :], MASK[:, :]), r=[MASK], w=[MASKB])
            for h in range(2):
                P.op("pe", lambda e, h=h: e.matmul(psW[h][:, :], UTB, MASKB[:, h * 512:(h + 1) * 512],
                                                    start=True, stop=True), r=[CSTB, MASKB], w=[psW[h]])
                P.op("pe", lambda e, h=h: e.matmul(psC[h][:, :], ONB, MASKB[:, h * 512:(h + 1) * 512],
                                                    start=True, stop=True), r=[CSTB, MASKB], w=[psC[h]])
                P.op("act", lambda e, h=h: e.copy(CNT[:, h * 512:(h + 1) * 512], psC[h][:, :]), r=[psC[h]], w=[CNT])
            P.op("dve", lambda e: e.memset(OFF[:, 0:NE], 0.0), w=[OFF])
            for t in range(1, NTILE):
                P.op("dve", lambda e, t=t: e.tensor_tensor(
                    OFF[:, t * NE:(t + 1) * NE], OFF[:, (t - 1) * NE:t * NE], CNT[:, (t - 1) * NE:t * NE], ALU.add),
                    r=[OFF, CNT], w=[OFF])
            for h in range(2):
                P.op("dve", lambda e, h=h: e.tensor_tensor(
                    POSM[:, h * 512:(h + 1) * 512], psW[h][:, :], OFF[:, h * 512:(h + 1) * 512], ALU.add),
                    r=[psW[h], OFF], w=[POSM])
            P.op("dve", lambda e: e.scalar_tensor_tensor(POSM[:, :], POSM[:, :], 1.0, MASK[:, :], ALU.add, ALU.mult),
                 r=[POSM, MASK], w=[POSM])
            P.op("dve", lambda e: e.tensor_scalar(POSM[:, :], POSM[:, :], -1.0, None, ALU.add), r=[POSM], w=[POSM])
            P.op("dve", lambda e: e.tensor_tensor(SL[:, :], POSM[:, :], cs("ecap"), ALU.add), r=[POSM, CST], w=[SL])
            for T in range(NTILE):
                for j in range(4):
                    c = T * 4 + j
                    P.op("dve", lambda e, T=T, j=j, c=c: e.scalar_tensor_tensor(
                        TMP[:, :], LOG[:, T * NE:(T + 1) * NE], MX8[:, T * 8 + j:T * 8 + j + 1],
                        SL[:, T * NE:(T + 1) * NE], ALU.is_equal, ALU.mult),
                        r=[LOG, MX8, SL], w=[TMP])
                    P.op("dve", lambda e, c=c: e.reduce_sum(SLJ[:, c:c + 1], TMP[:, :], AX.X), r=[TMP], w=[SLJ])
                    P.op("dve", lambda e, T=T, j=j, c=c: e.scalar_tensor_tensor(
                        TMP[:, :], LOG[:, T * NE:(T + 1) * NE], MX8[:, T * 8 + j:T * 8 + j + 1],
                        GATE[:, T * NE:(T + 1) * NE], ALU.is_equal, ALU.mult),
                        r=[LOG, MX8, GATE], w=[TMP])
                    P.op("dve", lambda e, c=c: e.reduce_sum(GJ[:, c:c + 1], TMP[:, :], AX.X), r=[TMP], w=[GJ])
            P.op("dve", lambda e: e.tensor_scalar(SLJ[:, :], SLJ[:, :], float(NE * CAP - 1), 0.0, ALU.min, ALU.max),
                 r=[SLJ], w=[SLJ])
            P.op("dve", lambda e: e.tensor_copy(SLJI[:, :], SLJ[:, :]), r=[SLJ], w=[SLJI])
            n = 0
            for ex in range(NE):
                pI = psI[ex % 2]
                for c in range(NCH):
                    P.op("pe", lambda e, c=c, pI=pI: e.matmul(
                        pI[:, c * 2:c * 2 + 2], ZB[:, :], TPB[:, 0:2], start=True, stop=False), r=[ZB, TPB], w=[pI])
                for t in range(NTILE):
                    oh = OH[n % 2]
                    n += 1
                    P.op("dve", lambda e, oh=oh, t=t, ex=ex: e.tensor_scalar(
                        oh[:, :], cs("iota"), POSM[:, t * NE + ex:t * NE + ex + 1], None, ALU.is_equal),
                        r=[CST, POSM], w=[oh])
                    for c in range(NCH):
                        P.op("pe", lambda e, oh=oh, t=t, c=c, pI=pI: e.matmul(
                            pI[:, c * 2:c * 2 + 2], oh[:, c * 128:(c + 1) * 128], TPB[:, t * 2:t * 2 + 2],
                            start=False, stop=(t == NTILE - 1)), r=[oh, TPB], w=[pI])
                tk = TK[ex % 2]
                P.op("act", lambda e, pI=pI, tk=tk: e.copy(tk[:, :], pI[:, 0:2 * NCH]), r=[pI], w=[tk])
                tkv = tk[:, :].rearrange("p (c two) -> p c two", two=2)
                P.op("dve", lambda e, tkv=tkv, ex=ex: e.scalar_tensor_tensor(
                    TOKF[:, ex * NCH:(ex + 1) * NCH], tkv[:, :, 0], 128.0, tkv[:, :, 1],
                    ALU.mult, ALU.add), r=[tk], w=[TOKF])
            P.op("dve", lambda e: e.tensor_copy(TOKI[:, :], TOKF[:, :]), r=[TOKF], w=[TOKI])
            P.barrier()
            P.flush()

        with ExitStack() as S3:
            WGU = [sb(S3, "WGU%d" % i, [128, 8, 2 * D], BF16) for i in range(2)]
            WD = [sb(S3, "WD%d" % i, [128, 8, D], BF16) for i in range(2)]
            BGU = sb(S3, "BGU", [128, NE * 16], F32)
            BD = [sb(S3, "BD%d" % i, [128, D], F32) for i in range(2)]
            XG = [sb(S3, "XG%d" % i, [128, D], BF16) for i in range(2)]
            XGT = sb(S3, "XGT", [128, 8, CAP], BF16)
            ACTT = sb(S3, "ACTT", [128, 8, CAP], BF16)
            G1t = [sb(S3, "G1t%d" % i, [128, 320], F32) for i in range(2)]
            SGt = [sb(S3, "SGt%d" % i, [128, 320], F32) for i in range(2)]
            U1t = [sb(S3, "U1t%d" % i, [128, 320], F32) for i in range(2)]
            YS = [sb(S3, "YS%d" % i, [128, D], F32) for i in range(2)]
            psX = ps(S3, "psX", [128, D], BF16)
            psG = [ps(S3, "psG%d" % i, [128, 512]) for i in range(2)]
            psU = [ps(S3, "psU%d" % i, [128, 512]) for i in range(2)]
            psY = [ps(S3, "psY%d" % i, [128, 512]) for i in range(2)]
            P.dma("sp", lambda e: e.dma_start(out=BGU[:, :], in_=bgu), w=[BGU])

            order = list(range(NE))
            WAa = WA.ap()

            WST = [sb(S3, "WST%d" % i, [128, 2], F32) for i in range(2)]
            nst = [0]

            def load_w(n):
                ex = order[n]
                wb = n % 2
                for k in range(8):
                    r0 = ex * D + k * 128
                    if ag:
                        P.dma("sp", lambda e, k=k, wb=wb, r0=r0: e.dma_start(
                            out=WGU[wb][:, k, :], in_=WAa[r0:r0 + 128, 0:2 * D]), r=["WAd"], w=[("WGU", wb, k)])
                        P.dma("act", lambda e, k=k, wb=wb, r0=r0: e.dma_start(
                            out=WD[wb][:, k, :], in_=WAa[r0:r0 + 128, 2 * D:3 * D]), r=["WAd"], w=[("WD", wb, k)])
                    else:
                        P.dma("pool", lambda e, k=k, wb=wb, ex=ex: e.dma_start(
                            out=WGU[wb][:, k, :], in_=wgu[ex, k * 128:(k + 1) * 128, :]), w=[("WGU", wb, k)])
                        P.dma("pool", lambda e, k=k, wb=wb, ex=ex: e.dma_start(
                            out=WD[wb][:, k, :], in_=wd[ex, k * 128:(k + 1) * 128, :]), w=[("WD", wb, k)])
                P.dma("sp", lambda e, wb=wb, ex=ex: e.dma_start(
                    out=BD[wb][:, :], in_=bd[ex, :].partition_broadcast(128)), w=[BD[wb]])

            def load_w_stage(n, idx):
                if True:
                    return
                ex = order[n]
                wb = n % 2
                k = 2 * (idx % 4) + 1
                st_ = WST[nst[0] % 2]
                nst[0] += 1
                if idx < 4:
                    P.dma("sp", lambda e, st_=st_, ex=ex, k=k: e.dma_start(
                        out=st_[:, :], in_=wgu[ex, k * 128:(k + 1) * 128, :]), w=[st_])
                    P.op("act", lambda e, st_=st_, wb=wb, k=k: e.copy(WGU[wb][:, k, :], st_[:, :]),
                         r=[st_], w=[("WGU", wb, k)])
                else:
                    P.dma("sp", lambda e, st_=st_, ex=ex, k=k: e.dma_start(
                        out=st_[:, 0:D], in_=wd[ex, k * 128:(k + 1) * 128, :]), w=[st_])
                    P.op("act", lambda e, st_=st_, wb=wb, k=k: e.copy(WD[wb][:, k, :], st_[:, 0:D]),
                         r=[st_], w=[("WD", wb, k)])

            for idx_ in range(8):
                load_w_stage(0, idx_)
            load_w(0)
            nact = 0
            for n_ in range(NE):
                ex = order[n_]
                wb = n_ % 2
                for c in range(NCH):
                    xg = XG[c % 2]
                    col = ex * NCH + c
                    P.dma("pool", lambda e, xg=xg, col=col: e.indirect_dma_start(
                        out=xg[:, :], out_offset=None, in_=XM[:, :],
                        in_offset=bass.IndirectOffsetOnAxis(ap=TOKI[:, col:col + 1], axis=0)),
                        r=[TOKI, "XMd"], w=[xg])
                    for k in range(8):
                        P.op("pe", lambda e, xg=xg, k=k: e.transpose(
                            psX[:, k * 128:(k + 1) * 128], xg[:, k * 128:(k + 1) * 128], IDB),
                            r=[xg, CSTB], w=[psX])
                    P.op("act", lambda e, c=c: e.copy(
                        XGT[:, :, c * 128:(c + 1) * 128], psX[:, :].rearrange("p (k t) -> p k t", k=8)),
                        r=[psX], w=[("XGT", c)])
                if n_ + 1 < NE:
                    load_w(n_ + 1)
                for j in range(8):
                    if n_ + 1 < NE:
                        load_w_stage(n_ + 1, j)
                    for h in range(2):
                        pg = psG[nact % 2]
                        pu = psU[nact % 2]
                        g1, sg, u1 = G1t[nact % 2], SGt[nact % 2], U1t[nact % 2]
                        nact += 1
                        xr = [("XGT", c) for c in range(NCH)]
                        for k in range(8):
                            P.op("pe", lambda e, pg=pg, wb=wb, k=k, j=j, h=h: e.matmul(
                                pg[:, 0:320], WGU[wb][:, k, j * 128:(j + 1) * 128], XGT[:, k, h * 320:(h + 1) * 320],
                                start=(k == 0), stop=(k == 7)), r=[("WGU", wb, k)] + xr, w=[pg])
                        for k in range(8):
                            P.op("pe", lambda e, pu=pu, wb=wb, k=k, j=j, h=h: e.matmul(
                                pu[:, 0:320], WGU[wb][:, k, D + j * 128:D + (j + 1) * 128],
                                XGT[:, k, h * 320:(h + 1) * 320],
                                start=(k == 0), stop=(k == 7)), r=[("WGU", wb, k)] + xr, w=[pu])
                        bg = BGU[:, ex * 16 + j:ex * 16 + j + 1]
                        bu = BGU[:, ex * 16 + 8 + j:ex * 16 + 8 + j + 1]
                        P.op("dve", lambda e, pg=pg, g1=g1, bg=bg: e.tensor_scalar(
                            g1[:, :], pg[:, 0:320], bg, 7.0, ALU.add, ALU.min), r=[pg, BGU], w=[g1])
                        P.op("act", lambda e, g1=g1, sg=sg: e.activation(sg[:, :], g1[:, :], AF.Sigmoid, scale=1.702),
                             r=[g1], w=[sg])
                        P.op("dve", lambda e, pu=pu, u1=u1, bu=bu: e.tensor_scalar(
                            u1[:, :], pu[:, 0:320], bu, 7.0, ALU.add, ALU.min), r=[pu, BGU], w=[u1])
                        P.op("dve", lambda e, u1=u1: e.tensor_scalar(
                            u1[:, :], u1[:, :], -7.0, 1.0, ALU.max, ALU.add), r=[u1], w=[u1])
                        P.op("dve", lambda e, g1=g1, sg=sg: e.tensor_tensor(g1[:, :], g1[:, :], sg[:, :], ALU.mult),
                             r=[g1, sg], w=[g1])
                        P.op("dve", lambda e, g1=g1, u1=u1, j=j, h=h: e.tensor_tensor(
                            ACTT[:, j, h * 320:(h + 1) * 320], g1[:, :], u1[:, :], ALU.mult),
                            r=[g1, u1], w=[("ACTT", j)])
                for c in range(NCH):
                    ys = YS[c % 2]
                    for h in range(2):
                        py = psY[h]
                        for j in range(8):
                            P.op("pe", lambda e, py=py, wb=wb, j=j, c=c, h=h: e.matmul(
                                py[:, :], ACTT[:, j, c * 128:(c + 1) * 128], WD[wb][:, j, h * 512:(h + 1) * 512],
                                start=(j == 0), stop=(j == 7)), r=[("ACTT", j), ("WD", wb, j)], w=[py])
                        P.op("dve", lambda e, py=py, ys=ys, wb=wb, h=h: e.tensor_tensor(
                            ys[:, h * 512:(h + 1) * 512], py[:, :], BD[wb][:, h * 512:(h + 1) * 512], ALU.add),
                            r=[py, BD[wb]], w=[ys])
                    row = ex * CAP + c * 128
                    P.dma("sp", lambda e, ys=ys, row=row: e.dma_start(out=Y[row:row + 128, :], in_=ys[:, :]),
                          r=[ys], w=["Yd"])
            P.barrier()
            P.flush()

        with ExitStack() as S4:
            YG = [sb(S4, "YG%d" % i, [128, D], F32) for i in range(8)]
            XMr = [sb(S4, "XMr%d" % i, [128, D], F32) for i in range(2)]
            ACC = [sb(S4, "ACC%d" % i, [128, D], F32) for i in range(2)]
            XO = [sb(S4, "XO%d" % i, [128, D], F32) for i in range(2)]
            outs = []
            for T in range(NTILE):
                b = T % 2
                P.dma("sp", lambda e, T=T, b=b: e.dma_start(out=XMr[b][:, :], in_=XM[T * 128:(T + 1) * 128, :]),
                      r=["XMd"], w=[XMr[b]])
                for j in range(4):
                    yg = YG[(T % 2) * 4 + j]
                    c = T * 4 + j
                    P.dma("pool", lambda e, yg=yg, c=c: e.indirect_dma_start(
                        out=yg[:, :], out_offset=None, in_=Y[:, :],
                        in_offset=bass.IndirectOffsetOnAxis(ap=SLJI[:, c:c + 1], axis=0)),
                        r=[SLJI, "Yd"], w=[yg])
                acc = ACC[b]
                P.op("dve", lambda e, acc=acc, b=b: e.tensor_scalar(acc[:, :], XMr[b][:, :], ALPHA, None, ALU.mult),
                     r=[XMr[b]], w=[acc])
                for j in range(4):
                    yg = YG[(T % 2) * 4 + j]
                    c = T * 4 + j
                    P.op("dve", lambda e, acc=acc, yg=yg, c=c: e.scalar_tensor_tensor(
                        acc[:, :], yg[:, :], GJ[:, c:c + 1], acc[:, :], ALU.mult, ALU.add),
                        r=[yg, GJ, acc], w=[acc])
                _layer_norm(P, acc, XO[b], G2, B2, LT)
                tok = P.dma("sp", lambda e, T=T, b=b: e.dma_start(out=xout[T * 128:(T + 1) * 128, :], in_=XO[b][:, :]),
                            r=[XO[b]], w=["xoutd"])
            P.barrier()
            P.flush()
    return nc, cst_np


def _consts_A():
    ident = np.eye(128, dtype=np.float32)
    ones = np.ones((128, 128), np.float32)
    tri = np.triu(np.ones((128, 128), np.float32), 0)
    return np.concatenate([ident, ones, tri], axis=1)


def _alibi_aug(heads):
    pos = np.arange(SEQ)
    hi = (pos // 64).astype(np.float32)
    lo = (pos % 64).astype(np.float32)
    qaug = np.stack([-hi, -lo, np.ones(SEQ, np.float32), np.ones(SEQ, np.float32)])
    kaug = np.zeros((len(heads), 4, SEQ), np.float32)
    for i, h in enumerate(heads):
        slope = 2.0 ** (-8.0 * (h + 1) / 8)
        kaug[i, 0] = slope * 64
        kaug[i, 1] = slope
        kaug[i, 2] = slope * 64 * hi
        kaug[i, 3] = slope * lo
    return _bf16(qaug), _bf16(kaug)


def build_phaseA_da(layer):
    lambda_init = 0.8 - 0.6 * math.exp(-0.3 * layer)
    nc = bass.Bass("TRN2", target_bir_lowering=False)

    def din(name, shape, dt):
        return nc.dram_tensor(name, shape, dt, kind="ExternalInput").ap()

    x = din("x", [SEQ, D], F32)
    wq = din("wq", [D, 512], F32)
    wk = din("wk", [D, 512], F32)
    wv = din("wv", [D, 512], F32)
    lam = din("lam", [4, 64], F32)
    sgin = din("sg", [128, 1], F32)
    qaug = din("qaug", [4, SEQ], BF16)
    kaug = din("kaug", [4, 4, SEQ], BF16)
    cst = din("cstA", [128, 384], F32)
    oT = nc.dram_tensor("oT", [4, 128, SEQ], BF16, kind="ExternalOutput").ap()
    XT = nc.dram_tensor("XT", [8, 128, SEQ], BF16, kind="Internal").ap()
    XTv = XT.rearrange("k p t -> p k t")
    NCHK = SEQ // 512

    with ExitStack() as S0:
        P = Prog(nc, S0)

        def sb(stack, name, shape, dt):
            return stack.enter_context(nc.sbuf_tensor(name, shape, dt))

        def ps(stack, name, shape, dt=F32):
            return stack.enter_context(nc.psum_tensor(name, shape, dt))

        CST = sb(S0, "CST", [128, 384], F32)
        CSTB = sb(S0, "CSTB", [128, 384], BF16)
        P.dma("sp", lambda e: e.dma_start(out=CST[:, :], in_=cst), w=[CST])
        P.dma("pool", lambda e: e.dma_start(out=CSTB[:, :], in_=cst), w=[CSTB])
        IDB, ONB, TRB = CSTB[:, 0:128], CSTB[:, 128:256], CSTB[:, 256:384]
        ONF = CST[:, 128:256]

        with ExitStack() as S1:
            XB = [sb(S1, "XB%d" % i, [128, D], BF16) for i in range(2)]
            XTg = [sb(S1, "XTg%d" % i, [128, 8, 512], BF16) for i in range(2)]
            psX = [ps(S1, "psX%d" % i, [128, D], BF16) for i in range(2)]
            for s in range(NCHK):
                g = XTg[s % 2]
                for t in range(4):
                    T = s * 4 + t
                    xb = XB[T % 2]
                    px = psX[T % 2]
                    P.dma("pool", lambda e, xb=xb, T=T: e.dma_start(out=xb[:, :], in_=x[T * 128:(T + 1) * 128, :]),
                          w=[xb])
                    for k in range(8):
                        P.op("pe", lambda e, px=px, xb=xb, k=k: e.transpose(
                            px[:, k * 128:(k + 1) * 128], xb[:, k * 128:(k + 1) * 128], IDB), r=[xb, CSTB], w=[px])
                    if t % 2 == 0:
                        P.op("act", lambda e, g=g, px=px, t=t: e.copy(
                            g[:, :, t * 128:(t + 1) * 128], px[:, :].rearrange("p (k t) -> p k t", k=8)),
                            r=[px], w=[g])
                    else:
                        P.op("dve", lambda e, g=g, px=px, t=t: e.tensor_copy(
                            g[:, :, t * 128:(t + 1) * 128], px[:, :].rearrange("p (k t) -> p k t", k=8)),
                            r=[px], w=[g])
                P.dma("sp", lambda e, g=g, s=s: e.dma_start(out=XTv[:, :, s * 512:(s + 1) * 512], in_=g[:, :, :]),
                      r=[g], w=["XTd"])
            P.barrier()
            P.flush()

        with ExitStack() as S2:
            WQ = sb(S2, "WQ", [128, 8, 512], BF16)
            WK = sb(S2, "WK", [128, 8, 512], BF16)
            WV = sb(S2, "WV", [128, 8, 512], BF16)
            QA = [sb(S2, "QA%d" % m, [68, SEQ], BF16) for m in range(2)]
            KA = [sb(S2, "KA%d" % m, [68, SEQ], BF16) for m in range(2)]
            V = sb(S2, "V", [128, SEQ // 128, 128], BF16)
            XTc = [sb(S2, "XTc%d" % i, [128, 8, 512], BF16) for i in range(2)]
            PT = [[sb(S2, "PT%d_%d" % (m, i), [128, 512], BF16) for i in range(2)] for m in range(2)]
            LQ = [sb(S2, "LQ%d" % i, [128, 64], F32) for i in range(4)]
            LS = sb(S2, "LS", [128, 4], F32)
            NLAM = sb(S2, "NLAM", [128, 1], F32)
            GSC = sb(S2, "GSC", [128, 1], F32)
            EPS = sb(S2, "EPS", [128, 1], F32)
            R1 = sb(S2, "R1", [128, 512], F32)
            R2 = sb(S2, "R2", [128, 512], F32)
            OA = sb(S2, "OA", [128, 512], F32)
            OB = sb(S2, "OB", [128, 512], F32)
            SQ = sb(S2, "SQ", [128, 512], F32)
            OTt = [sb(S2, "OTt%d" % i, [128, 512], BF16) for i in range(2)]
            psS = [[ps(S2, "psS%d_%d" % (m, i), [128, 512]) for i in range(2)] for m in range(2)]
            psO = [ps(S2, "psO%d" % m, [128, 512]) for m in range(2)]
            psZ = [ps(S2, "psZ%d" % m, [128, 512]) for m in range(2)]

            for W, src in ((WQ, wq), (WK, wk), (WV, wv)):
                for k in range(8):
                    P.dma("pool", lambda e, W=W, src=src, k=k: e.dma_start(
                        out=W[:, k, :], in_=src[k * 128:(k + 1) * 128, :]), w=[W])
            for m in range(2):
                P.dma("sp", lambda e, m=m: e.dma_start(out=QA[m][64:68, :], in_=qaug), w=[QA[m]])
            for i in range(4):
                P.dma("sp", lambda e, i=i: e.dma_start(out=LQ[i][:, :], in_=lam[i, :].partition_broadcast(128)),
                      w=[LQ[i]])
            P.dma("sp", lambda e: e.dma_start(out=GSC[:, :], in_=sgin), w=[GSC])
            P.op("dve", lambda e: e.memset(EPS[:, :], LN_EPS), w=[EPS])
            for j in range(2):
                P.op("dve", lambda e, j=j: e.tensor_tensor(LQ[2 * j][:, :], LQ[2 * j][:, :], LQ[2 * j + 1][:, :], ALU.mult),
                     r=[LQ[2 * j], LQ[2 * j + 1]], w=[LQ[2 * j]])
                P.op("dve", lambda e, j=j: e.reduce_sum(LS[:, j:j + 1], LQ[2 * j][:, :], AX.X), r=[LQ[2 * j]], w=[LS])
            P.op("act", lambda e: e.activation(LS[:, 2:4], LS[:, 0:2], AF.Exp), r=[LS], w=[LS])
            P.op("dve", lambda e: e.tensor_tensor(NLAM[:, :], LS[:, 3:4], LS[:, 2:3], ALU.subtract), r=[LS], w=[NLAM])
            P.op("dve", lambda e: e.tensor_scalar(NLAM[:, :], NLAM[:, :], -lambda_init, None, ALU.add),
                 r=[NLAM], w=[NLAM])
            P.op("dve", lambda e: e.tensor_scalar(GSC[:, :], GSC[:, :], 1.0 - lambda_init, None, ALU.mult),
                 r=[GSC], w=[GSC])

            npt = 0
            for h in range(4):
                for m in range(2):
                    P.dma("sp", lambda e, m=m, h=h: e.dma_start(out=KA[m][64:68, :], in_=kaug[h, :, :]), w=[KA[m]])
                for s in range(NCHK):
                    xc = XTc[s % 2]
                    P.dma("sp", lambda e, xc=xc, s=s: e.dma_start(out=xc[:, :, :], in_=XTv[:, :, s * 512:(s + 1) * 512]),
                          r=["XTd"], w=[xc])
                    for (W, dst, sc) in ((WQ, QA, 0.125), (WK, KA, 1.0)):
                        for m in range(2):
                            pp = psS[m][s % 2]
                            c0 = h * 128 + m * 64
                            for k in range(8):
                                P.op("pe", lambda e, pp=pp, W=W, k=k, c0=c0, xc=xc: e.matmul(
                                    pp[0:64, :], W[:, k, c0:c0 + 64], xc[:, k, :], start=(k == 0), stop=(k == 7)),
                                    r=[W, xc], w=[pp])
                            P.op("act", lambda e, pp=pp, dst=dst, m=m, s=s, sc=sc: e.activation(
                                dst[m][0:64, s * 512:(s + 1) * 512], pp[0:64, :], AF.Copy, scale=sc),
                                r=[pp], w=[dst[m]])
                    for t in range(4):
                        pp = psO[t % 2]
                        for k in range(8):
                            P.op("pe", lambda e, pp=pp, k=k, t=t, xc=xc, h=h: e.matmul(
                                pp[:, 0:128], xc[:, k, t * 128:(t + 1) * 128], WV[:, k, h * 128:(h + 1) * 128],
                                start=(k == 0), stop=(k == 7)), r=[WV, xc], w=[pp])
                        P.op("dve", lambda e, pp=pp, s=s, t=t: e.tensor_copy(V[:, s * 4 + t, :], pp[:, 0:128]),
                             r=[pp], w=[V])
                def _finalize(i, h=h):
                    P.op("dve", lambda e: e.reciprocal(R1[:, :], psZ[0][:, :]), r=[psZ[0]], w=[R1])
                    P.op("dve", lambda e: e.reciprocal(R2[:, :], psZ[1][:, :]), r=[psZ[1]], w=[R2])
                    P.op("dve", lambda e: e.tensor_tensor(OA[:, :], psO[0][:, :], R1[:, :], ALU.mult), r=[psO[0], R1], w=[OA])
                    P.op("dve", lambda e: e.tensor_tensor(OB[:, :], psO[1][:, :], R2[:, :], ALU.mult), r=[psO[1], R2], w=[OB])
                    P.op("dve", lambda e: e.scalar_tensor_tensor(OA[:, :], OB[:, :], NLAM[:, 0:1], OA[:, :], ALU.mult, ALU.add),
                         r=[OA, OB, NLAM], w=[OA])
                    P.op("dve", lambda e: e.tensor_tensor(SQ[:, :], OA[:, :], OA[:, :], ALU.mult), r=[OA], w=[SQ])
                    pR = psZ[0]
                    P.op("pe", lambda e, pR=pR: e.matmul(pR[:, :], ONF, SQ[:, :], start=True, stop=True), r=[CST, SQ], w=[pR])
                    P.op("act", lambda e, pR=pR: e.activation(R1[:, :], pR[:, :], AF.Sqrt, bias=EPS[:, 0:1], scale=1.0 / 128),
                         r=[pR, EPS], w=[R1])
                    P.op("dve", lambda e: e.reciprocal(R1[:, :], R1[:, :]), r=[R1], w=[R1])
                    P.op("dve", lambda e: e.tensor_tensor(OA[:, :], OA[:, :], R1[:, :], ALU.mult), r=[OA, R1], w=[OA])
                    ot = OTt[i % 2]
                    P.op("dve", lambda e, ot=ot: e.tensor_scalar(ot[:, :], OA[:, :], GSC[:, 0:1], None, ALU.mult),
                         r=[OA, GSC], w=[ot])
                    P.dma("sp", lambda e, ot=ot, h=h, i=i: e.dma_start(out=oT[h, :, i * 512:(i + 1) * 512], in_=ot[:, :]),
                          r=[ot], w=["oTd"])
                its = [(i, kb) for i in range(NCHK) for kb in range(4 * i + 4)]
                pend = None
                for idx in range(len(its) + 1):
                    cur = None
                    if idx < len(its):
                        i, kb = its[idx]
                        jl = kb - 4 * i
                        c0 = 128 * jl if jl > 0 else 0
                        pts = []
                        for m in range(2):
                            pS = psS[m][npt % 2]
                            pt = PT[m][npt % 2]
                            pts.append(pt)
                            P.op("pe", lambda e, pS=pS, m=m, kb=kb, i=i, c0=c0: e.matmul(
                                pS[:, c0:512], KA[m][0:68, kb * 128:(kb + 1) * 128],
                                QA[m][0:68, i * 512 + c0:(i + 1) * 512], start=True, stop=True),
                                r=[KA[m], QA[m]], w=[pS])
                            P.op("act", lambda e, pS=pS, pt=pt, c0=c0: e.activation(pt[:, c0:512], pS[:, c0:512], AF.Exp),
                                 r=[pS], w=[pt])
                            if jl >= 0:
                                P.op("dve", lambda e, pt=pt, c0=c0: e.tensor_tensor(
                                    pt[:, c0:c0 + 128], pt[:, c0:c0 + 128], TRB, ALU.mult), r=[pt, CSTB], w=[pt])
                        npt += 1
                        cur = (i, kb, c0, pts)
                    if pend is not None:
                        i, kb, c0, pts = pend
                        nkb = 4 * i + 4
                        for m in range(2):
                            pt = pts[m]
                            P.op("pe", lambda e, m=m, pt=pt, kb=kb, c0=c0, nkb=nkb: e.matmul(
                                psO[m][:, c0:512], V[:, kb, :], pt[:, c0:512], start=(kb == 0), stop=(kb == nkb - 1)),
                                r=[V, pt], w=[psO[m]])
                            P.op("pe", lambda e, m=m, pt=pt, kb=kb, c0=c0, nkb=nkb: e.matmul(
                                psZ[m][:, c0:512], ONB, pt[:, c0:512], start=(kb == 0), stop=(kb == nkb - 1)),
                                r=[CSTB, pt], w=[psZ[m]])
                        if kb == nkb - 1:
                            _finalize(i)
                    pend = cur
            P.barrier()
            P.flush()
    return nc


def _ret_consts(heads):
    C = 128
    out = np.zeros((len(heads), 128, 128 + 512 + 2), np.float32)
    pos = np.arange(C, dtype=np.float64)
    for i, h in enumerate(heads):
        lg = math.log1p(-2.0 ** (-5.0 - h))
        diff = pos[None, :] - pos[:, None]
        dmT = np.where(diff >= 0, np.exp(lg * np.maximum(diff, 0.0)), 0.0)
        out[i, :, 0:128] = dmT
        qd = np.exp(lg * (pos + 1.0))
        out[i, :, 128:640] = np.tile(qd, 4)[None, :]
        out[i, :, 640] = np.exp(lg * (C - 1.0 - pos)) / 16.0
        out[i, :, 641] = math.exp(lg * C)
    return out


def build_phaseA_ret(dbg_nh=2, dbg_nchk=None, dbg_stage=9):
    nc = bass.Bass("TRN2", target_bir_lowering=False)

    def din(name, shape, dt):
        return nc.dram_tensor(name, shape, dt, kind="ExternalInput").ap()

    x = din("x", [SEQ, D], F32)
    wq = din("wq", [D, 512], F32)
    wk = din("wk", [D, 512], F32)
    wv = din("wv", [D, 1024], F32)
    wg = din("wg", [D, 1024], F32)
    rdec = din("rdec", [2, 128, 642], F32)
    cst = din("cstA", [128, 384], F32)
    oT = nc.dram_tensor("oT", [8, 128, SEQ], BF16, kind="ExternalOutput").ap()
    XT = nc.dram_tensor("XT", [8, 128, SEQ], BF16, kind="Internal").ap()
    XTv = XT.rearrange("k p t -> p k t")
    oTv = oT.rearrange("k p t -> p k t")
    NCHK = SEQ // 512

    with ExitStack() as S0:
        P = Prog(nc, S0)

        def sb(stack, name, shape, dt):
            return stack.enter_context(nc.sbuf_tensor(name, shape, dt))

        def ps(stack, name, shape, dt=F32):
            return stack.enter_context(nc.psum_tensor(name, shape, dt))

        CSTB = sb(S0, "CSTB", [128, 384], BF16)
        P.dma("pool", lambda e: e.dma_start(out=CSTB[:, :], in_=cst), w=[CSTB])
        IDB = CSTB[:, 0:128]

        with ExitStack() as S1:
            XB = [sb(S1, "XB%d" % i, [128, D], BF16) for i in range(2)]
            XTg = [sb(S1, "XTg%d" % i, [128, 8, 512], BF16) for i in range(2)]
            psX = [ps(S1, "psX%d" % i, [128, D], BF16) for i in range(2)]
            for s in range(NCHK if dbg_nchk is None else dbg_nchk):
                g = XTg[s % 2]
                for t in range(4):
                    T = s * 4 + t
                    xb = XB[T % 2]
                    px = psX[T % 2]
                    P.dma("pool", lambda e, xb=xb, T=T: e.dma_start(out=xb[:, :], in_=x[T * 128:(T + 1) * 128, :]),
                          w=[xb])
                    for k in range(8):
                        P.op("pe", lambda e, px=px, xb=xb, k=k: e.transpose(
                            px[:, k * 128:(k + 1) * 128], xb[:, k * 128:(k + 1) * 128], IDB), r=[xb, CSTB], w=[px])
                    if t % 2 == 0:
                        P.op("act", lambda e, g=g, px=px, t=t: e.copy(
                            g[:, :, t * 128:(t + 1) * 128], px[:, :].rearrange("p (k t) -> p k t", k=8)),
                            r=[px], w=[g])
                    else:
                        P.op("dve", lambda e, g=g, px=px, t=t: e.tensor_copy(
                            g[:, :, t * 128:(t + 1) * 128], px[:, :].rearrange("p (k t) -> p k t", k=8)),
                            r=[px], w=[g])
                P.dma("sp", lambda e, g=g, s=s: e.dma_start(out=XTv[:, :, s * 512:(s + 1) * 512], in_=g[:, :, :]),
                      r=[g], w=["XTd"])
            P.barrier()
            P.flush()

        with ExitStack() as S2:
            WQ = sb(S2, "WQ", [128, 8, 512], BF16)
            WK = sb(S2, "WK", [128, 8, 512], BF16)
            WV = sb(S2, "WV", [128, 8, 1024], BF16)
            WG = sb(S2, "WG", [128, 8, 1024], BF16)
            RD = [sb(S2, "RD%d" % i, [128, 642], F32) for i in range(2)]
            XTc = [sb(S2, "XTc%d" % i, [128, 8, 512], BF16) for i in range(2)]
            QT = [sb(S2, "QT%d" % i, [128, 512], BF16) for i in range(2)]
            QTd = [sb(S2, "QTd%d" % i, [128, 512], BF16) for i in range(2)]
            KT = [sb(S2, "KT%d" % i, [128, 512], BF16) for i in range(2)]
            Kd = [sb(S2, "Kd%d" % i, [128, 256], BF16) for i in range(4)]
            Vt = [sb(S2, "Vt%d" % i, [128, 512], BF16) for i in range(4)]
            SG = [sb(S2, "SG%d" % i, [128, 512], F32) for i in range(4)]
            ST = [sb(S2, "ST%d" % i, [128, 512], F32) for i in range(2)]
            STb = [sb(S2, "STb%d" % i, [128, 512], BF16) for i in range(2)]
            SCT = [sb(S2, "SCT%d" % i, [128, 128], BF16) for i in range(2)]
            ON = [sb(S2, "ON%d" % i, [128, 512], F32) for i in range(2)]
            OG = [sb(S2, "OG%d" % i, [128, 512], BF16) for i in range(2)]
            OTg = [sb(S2, "OTg%d" % i, [128, 4, 512], BF16) for i in range(2)]
            st = sb(S2, "st", [128, 6], F32)
            mv = sb(S2, "mv", [128, 2], F32)
            rstd = sb(S2, "rstd", [128, 1], F32)
            EPS = sb(S2, "EPS", [128, 1], F32)
            psP = [ps(S2, "psP%d" % i, [128, 512]) for i in range(2)]
            psS = ps(S2, "psS", [128, 512])
            psO = ps(S2, "psO", [128, 512])
            psU = [ps(S2, "psU%d" % i, [128, 512]) for i in range(2)]
            psTr = ps(S2, "psTr", [128, 512], BF16)

            for W, src in ((WQ, wq), (WK, wk), (WV, wv), (WG, wg)):
                for k in range(8):
                    P.dma("pool", lambda e, W=W, src=src, k=k: e.dma_start(
                        out=W[:, k, :], in_=src[k * 128:(k + 1) * 128, :]), w=[W])
            for h in range(2):
                P.dma("sp", lambda e, h=h: e.dma_start(out=RD[h][:, :], in_=rdec[h, :, :]), w=[RD[h]])
            P.op("dve", lambda e: e.memset(EPS[:, :], LN_EPS), w=[EPS])
            npp = 0
            for h in range(dbg_nh):
                DMT = RD[h][:, 0:128]
                QD4 = RD[h][:, 128:640]
                KDC = RD[h][:, 640:641]
                CDC = RD[h][:, 641:642]
                for c2 in range(2):
                    P.op("dve", lambda e, c2=c2: e.memset(ST[c2][:, :], 0.0), w=[ST[c2]])
                    if 'mset' not in DBG_SKIP:
                        P.op("dve", lambda e, c2=c2: e.memset(STb[c2][:, :], 0.0), w=[STb[c2]])
                for s in range(NCHK if dbg_nchk is None else dbg_nchk):
                    xc = XTc[s % 2]
                    P.dma("sp", lambda e, xc=xc, s=s: e.dma_start(out=xc[:, :, :], in_=XTv[:, :, s * 512:(s + 1) * 512]),
                          r=["XTd"], w=[xc])
                    for c2 in range(2):
                        c0 = h * 256 + c2 * 128
                        pp = psP[npp % 2]; npp += 1
                        for k in range(8):
                            P.op("pe", lambda e, pp=pp, k=k, c0=c0, xc=xc: e.matmul(
                                pp[:, :], WQ[:, k, c0:c0 + 128], xc[:, k, :], start=(k == 0), stop=(k == 7)),
                                r=[WQ, xc], w=[pp])
                        P.op("dve", lambda e, pp=pp, c2=c2: e.tensor_copy(QT[c2][:, :], pp[:, :]), r=[pp], w=[QT[c2]])
                        if 'qtd' not in DBG_SKIP:
                            P.op("dve", lambda e, pp=pp, c2=c2, QD4=QD4: e.tensor_tensor(QTd[c2][:, :], pp[:, :], QD4, ALU.mult),
                                 r=[pp, RD[h]], w=[QTd[c2]])
                        pp = psP[npp % 2]; npp += 1
                        for k in range(8):
                            P.op("pe", lambda e, pp=pp, k=k, c0=c0, xc=xc: e.matmul(
                                pp[:, :], WK[:, k, c0:c0 + 128], xc[:, k, :], start=(k == 0), stop=(k == 7)),
                                r=[WK, xc], w=[pp])
                        if 'kt' not in DBG_SKIP:
                            P.op("act", lambda e, pp=pp, c2=c2: e.activation(KT[c2][:, :], pp[:, :], AF.Copy, scale=1.0 / 16),
                                 r=[pp], w=[KT[c2]])
                    for t in range(4):
                        pp = psP[npp % 2]; npp += 1
                        for k in range(8):
                            P.op("pe", lambda e, pp=pp, k=k, t=t, xc=xc, h=h: e.matmul(
                                pp[:, 0:256], xc[:, k, t * 128:(t + 1) * 128], WK[:, k, h * 256:(h + 1) * 256],
                                start=(k == 0), stop=(k == 7)), r=[WK, xc], w=[pp])
                        if 'kd' not in DBG_SKIP:
                            P.op("dve", lambda e, pp=pp, t=t, KDC=KDC: e.tensor_scalar(
                                Kd[t][:, :], pp[:, 0:256], KDC, None, ALU.mult), r=[pp, RD[h]], w=[Kd[t]])
                        pp = psP[npp % 2]; npp += 1
                        for k in range(8):
                            P.op("pe", lambda e, pp=pp, k=k, t=t, xc=xc, h=h: e.matmul(
                                pp[:, :], xc[:, k, t * 128:(t + 1) * 128], WV[:, k, h * 512:(h + 1) * 512],
                                start=(k == 0), stop=(k == 7)), r=[WV, xc], w=[pp])
                        P.op("act", lambda e, pp=pp, t=t: e.copy(Vt[t][:, :], pp[:, :]), r=[pp], w=[Vt[t]])
                        pp = psP[npp % 2]; npp += 1
                        for k in range(8):
                            P.op("pe", lambda e, pp=pp, k=k, t=t, xc=xc, h=h: e.matmul(
                                pp[:, :], xc[:, k, t * 128:(t + 1) * 128], WG[:, k, h * 512:(h + 1) * 512],
                                start=(k == 0), stop=(k == 7)), r=[WG, xc], w=[pp])
                        if 'sg' not in DBG_SKIP:
                            P.op("act", lambda e, pp=pp, t=t: e.activation(SG[t][:, :], pp[:, :], AF.Sigmoid), r=[pp], w=[SG[t]])
                        if 'sg' not in DBG_SKIP:
                            P.op("dve", lambda e, pp=pp, t=t: e.tensor_tensor(SG[t][:, :], SG[t][:, :], pp[:, :], ALU.mult),
                                 r=[pp, SG[t]], w=[SG[t]])
                    og4 = OTg[s % 2]
                    for t in range(4 if dbg_stage >= 2 else 0):
                        tt = slice(t * 128, (t + 1) * 128)
                        sct = SCT[t % 2]
                        for c2 in range(2):
                            P.op("pe", lambda e, c2=c2, tt=tt: e.matmul(
                                psS[:, 0:128], KT[c2][:, tt], QT[c2][:, tt], start=(c2 == 0), stop=(c2 == 1)),
                                r=[KT[c2], QT[c2]], w=[psS])
                        P.op("dve", lambda e, sct=sct, DMT=DMT: e.tensor_tensor(sct[:, :], psS[:, 0:128], DMT, ALU.mult),
                             r=[psS, RD[h]], w=[sct])
                        P.op("pe", lambda e, sct=sct, t=t: e.matmul(psO[:, :], sct[:, :], Vt[t][:, :], start=True, stop=False),
                             r=[sct, Vt[t]], w=[psO])
                        for c2 in range(2):
                            P.op("pe", lambda e, c2=c2, tt=tt: e.matmul(
                                psO[:, :], QTd[c2][:, tt], STb[c2][:, :], start=False, stop=(c2 == 1)),
                                r=[QTd[c2], STb[c2]], w=[psO])
                        for c2 in range(2 if dbg_stage >= 3 else 0):
                            P.op("pe", lambda e, c2=c2, t=t: e.matmul(
                                psU[c2][:, :], Kd[t][:, c2 * 128:(c2 + 1) * 128], Vt[t][:, :], start=True, stop=True),
                                r=[Kd[t], Vt[t]], w=[psU[c2]])
                            P.op("dve", lambda e, c2=c2, CDC=CDC: e.scalar_tensor_tensor(
                                ST[c2][:, :], ST[c2][:, :], CDC, psU[c2][:, :], ALU.mult, ALU.add),
                                r=[ST[c2], psU[c2], RD[h]], w=[ST[c2]])
                            P.op("act", lambda e, c2=c2: e.copy(STb[c2][:, :], ST[c2][:, :]), r=[ST[c2]], w=[STb[c2]])
                        on = ON[t % 2]
                        og = OG[t % 2]
                        if dbg_stage < 4:
                            continue
                        P.op("dve", lambda e: e.bn_stats(st[:, :], psO[:, :]), r=[psO], w=[st])
                        P.op("dve", lambda e: e.bn_aggr(mv[:, :], st[:, :]), r=[st], w=[mv])
                        P.op("act", lambda e: e.activation(rstd[:, :], mv[:, 1:2], AF.Sqrt, bias=EPS[:, 0:1], scale=1.0),
                             r=[mv, EPS], w=[rstd])
                        P.op("dve", lambda e: e.reciprocal(rstd[:, :], rstd[:, :]), r=[rstd], w=[rstd])
                        P.op("dve", lambda e, on=on: e.tensor_scalar(on[:, :], psO[:, :], mv[:, 0:1], rstd[:, 0:1],
                                                                     ALU.subtract, ALU.mult), r=[psO, mv, rstd], w=[on])
                        P.op("dve", lambda e, on=on, og=og, t=t: e.tensor_tensor(og[:, :], on[:, :], SG[t][:, :], ALU.mult),
                             r=[on, SG[t]], w=[og])
                        if dbg_stage < 5:
                            continue
                        for j in range(4):
                            P.op("pe", lambda e, og=og, j=j: e.transpose(
                                psTr[:, j * 128:(j + 1) * 128], og[:, j * 128:(j + 1) * 128], IDB), r=[og, CSTB], w=[psTr])
                        P.op("act", lambda e, og4=og4, tt=tt: e.copy(
                            og4[:, :, tt], psTr[:, :].rearrange("p (j t) -> p j t", j=4)), r=[psTr], w=[og4])
                    if dbg_stage >= 6:
                        P.dma("sp", lambda e, og4=og4, h=h, s=s: e.dma_start(
                            out=oTv[:, h * 4:(h + 1) * 4, s * 512:(s + 1) * 512], in_=og4[:, :, :]), r=[og4], w=["oTd"])
            P.barrier()
            P.flush()
    return nc


_PROGS = {}


def _prog(key, builder):
    if key not in _PROGS:
        _PROGS[key] = builder()
    return _PROGS[key]


def _run(nc, ims):
    return run_bass_kernel_spmd(nc, ims, core_ids=list(range(NCORE))).results


def _mixer_inputs(inp, i, X):
    j = i // 2
    cstA = _consts_A()
    ims = []
    for c in range(NCORE):
        b, hg = c // 2, c % 2
        xb = np.ascontiguousarray(X[b])
        if i % 2 == 0:
            w_in = inp["da_w_in"][j]
            heads = list(range(hg * 4, hg * 4 + 4))
            qaug, kaug = _alibi_aug(heads)
            ims.append({
                "x": xb,
                "wq": np.ascontiguousarray(w_in[:, hg * 512:(hg + 1) * 512]),
                "wk": np.ascontiguousarray(w_in[:, 1024 + hg * 512:1024 + (hg + 1) * 512]),
                "wv": np.ascontiguousarray(w_in[:, 2048 + hg * 512:2048 + (hg + 1) * 512]),
                "lam": np.stack([inp["da_lam_q1"][j], inp["da_lam_k1"][j], inp["da_lam_q2"][j], inp["da_lam_k2"][j]]),
                "sg": np.ascontiguousarray(inp["da_subln_g"][j].reshape(128, 1)),
                "qaug": qaug, "kaug": kaug, "cstA": cstA})
        else:
            w_in = inp["ret_w_in"][j]
            ims.append({
                "x": xb,
                "wq": np.ascontiguousarray(w_in[:, hg * 512:(hg + 1) * 512]),
                "wk": np.ascontiguousarray(w_in[:, 1024 + hg * 512:1024 + (hg + 1) * 512]),
                "wv": np.ascontiguousarray(w_in[:, 2048 + hg * 1024:2048 + (hg + 1) * 1024]),
                "wg": np.ascontiguousarray(w_in[:, 4096 + hg * 1024:4096 + (hg + 1) * 1024]),
                "rdec": _ret_consts([2 * hg, 2 * hg + 1]), "cstA": cstA})
    return ims


def _ffn_inputs(inp, i, X, oTs, cstB):
    j = i // 2
    w_out = inp["da_w_out"][j] if i % 2 == 0 else inp["ret_w_out"][j]
    lnp = np.stack([inp["ln_mix_g"][i], inp["ln_mix_b"][i], inp["ln_ffn_g"][i], inp["ln_ffn_b"][i]])
    bgu = np.ascontiguousarray(inp["moe_b_gate_up"][i].reshape(NE, 16, 128).transpose(2, 0, 1).reshape(128, NE * 16))
    ims = []
    wgu_full = np.ascontiguousarray(inp["moe_w_gate_up"][i])
    wd_full = np.ascontiguousarray(inp["moe_w_down"][i])
    for c in range(NCORE):
        b, half = c // 2, c % 2
        sl = slice(half * NT, (half + 1) * NT)
        oT = np.ascontiguousarray(np.concatenate([oTs[2 * b + hg][:, :, sl] for hg in range(2)], axis=0))
        ims.append({
            "xres": np.ascontiguousarray(X[b, sl]), "oT": oT, "wout": np.ascontiguousarray(w_out), "lnp": lnp,
            "wr": np.ascontiguousarray(inp["moe_w_router"][i]), "br": np.ascontiguousarray(inp["moe_b_router"][i][None, :]),
            "wgu": wgu_full, "bgu": bgu,
            "wd": wd_full,
            "bd": np.ascontiguousarray(inp["moe_b_down"][i]), "cst": cstB})
    return ims


def kernel(**inp):
    inp = {k: np.asarray(v) for k, v in inp.items()}
    X = np.array(inp["x"], dtype=np.float32, copy=True)
    for i in range(4):
        if i % 2 == 0:
            ncA = _prog(("da", i), lambda: build_phaseA_da(i))
            KC = 8
        else:
            ncA = _prog(("ret",), build_phaseA_ret)
            KC = 16
        resA = _run(ncA, _mixer_inputs(inp, i, X))
        oTs = [r["oT"] for r in resA]
        ncB, cstB = _prog(("B", KC), lambda: build_phaseB(KC))
        resB = _run(ncB, _ffn_inputs(inp, i, X, oTs, cstB))
        Xn = np.empty_like(X)
        for c in range(NCORE):
            b, half = c // 2, c % 2
            Xn[b, half * NT:(half + 1) * NT] = resB[c]["xout"]
        X = Xn
    return X
```
